# Optimizing a Trainium2 kernel written in Bass

```python
import math
import jax, jax.numpy as jnp
from jax import lax
import numpy as np

D_MODEL = 1024
BATCH = 8
SEQ = 4096
DEPTH = 1

CTX_LEN = 256
GRID_W = 64
EPS = 1e-6
DA_HEADS = 4
DA_HEAD_DIM = 64
DA_V_DIM = 2 * DA_HEAD_DIM
DA_WIDTH = DA_HEADS * DA_V_DIM
Q_BLOCK = 128
ROPE_THETA = 10000.0
GM_HEADS = 4
GM_HEAD_DIM = 128
GM_WIDTH = GM_HEADS * GM_HEAD_DIM
CHUNK = 128
MIX_WIDTH = DA_WIDTH + GM_WIDTH
IN_WIDTH = 3 * DA_WIDTH + 2 * GM_WIDTH
N_EXPERTS = 32
TOP_K = 4
N_GROUPS = 4
TOPK_GROUPS = 2
D_EXPERT = 256
D_SHARED = 256
ROUTED_SCALE = 2.5
MOE_BLOCK = 128

kernel_name = "hybrid_diffattn_chunkgmlp_moe_dit"


def rms_norm(x, g):
    xf = x.astype(jnp.float32)
    y = xf * lax.rsqrt(jnp.mean(xf * xf, axis=-1, keepdims=True) + EPS)
    return (y * g.astype(jnp.float32)).astype(x.dtype)


def layer_norm(x, g, b):
    xf = x.astype(jnp.float32)
    mu = jnp.mean(xf, axis=-1, keepdims=True)
    xc = xf - mu
    var = jnp.mean(xc * xc, axis=-1, keepdims=True)
    y = xc * lax.rsqrt(var + EPS) * g.astype(jnp.float32) + b.astype(jnp.float32)
    return y.astype(x.dtype)


def adaln_params(cond, w_ada, b_ada):
    m = jax.nn.silu(cond) @ w_ada + b_ada
    return jnp.split(m, 6, axis=-1)


def modulate(h, shift, scale):
    return h * (1.0 + scale) + shift


def axial_rope_tables(n_tokens):
    rows = n_tokens // GRID_W
    row = jnp.repeat(jnp.arange(rows, dtype=jnp.float32), GRID_W)
    col = jnp.tile(jnp.arange(GRID_W, dtype=jnp.float32), rows)
    half = DA_HEAD_DIM // 2
    inv_freq = ROPE_THETA ** (-jnp.arange(0, half, 2, dtype=jnp.float32) / half)
    ang = jnp.concatenate([row[:, None] * inv_freq, col[:, None] * inv_freq], axis=-1)
    return jnp.cos(ang), jnp.sin(ang)


def apply_rope(x, cos, sin):
    xf = x.astype(jnp.float32)
    x1, x2 = xf[..., 0::2], xf[..., 1::2]
    out = jnp.stack([x1 * cos - x2 * sin, x1 * sin + x2 * cos], axis=-1).reshape(x.shape)
    return out.astype(x.dtype)


def split_proj(h, w_in):
    B, L, _ = h.shape
    p = h @ w_in
    q = p[..., :DA_WIDTH].reshape(B, L, DA_HEADS, 2, DA_HEAD_DIM)
    k = p[..., DA_WIDTH:2 * DA_WIDTH].reshape(B, L, DA_HEADS, 2, DA_HEAD_DIM)
    v = p[..., 2 * DA_WIDTH:3 * DA_WIDTH].reshape(B, L, DA_HEADS, DA_V_DIM)
    z = jax.nn.gelu(p[..., 3 * DA_WIDTH:], approximate=False).reshape(B, L, 2, GM_HEADS, GM_HEAD_DIM)
    q = jnp.transpose(q, (0, 2, 3, 1, 4))
    k = jnp.transpose(k, (0, 2, 3, 1, 4))
    v = jnp.transpose(v, (0, 2, 1, 3))
    return q, k, v, z[:, :, 0], z[:, :, 1]


def diff_attend(q, k, v, lam):
    s = jnp.einsum('bhmqd,bhmkd->bhmqk', q, k).astype(jnp.float32) * (DA_HEAD_DIM ** -0.5)
    p = jax.nn.softmax(s, axis=-1)
    a = p[:, :, 0] - lam * p[:, :, 1]
    return jnp.einsum('bhqk,bhkv->bhqv', a.astype(v.dtype), v)


def da_output(o, subln_g, lambda_init):
    B, H, L, dv = o.shape
    o = rms_norm(o, subln_g) * (1.0 - lambda_init)
    return jnp.transpose(o, (0, 2, 1, 3)).reshape(B, L, H * dv)


def chunk_gmlp(u, vg, ln_g, ln_b, w_s, b_s, out_g):
    B, L, G, dh = vg.shape
    vn = layer_norm(vg, ln_g, ln_b)
    vc = vn.reshape(B, L // CHUNK, CHUNK, G, dh)
    mixed = jnp.einsum('gpq,bnqgd->bnpgd', w_s, vc) + b_s.T[:, :, None]
    y = u * mixed.reshape(B, L, G, dh)
    return rms_norm(y, out_g).reshape(B, L, G * dh)


def moe_ffn(h, w_router, router_bias, we_gate, we_up, we_down, ws_gate, ws_up, ws_down):
    shape = h.shape
    t = h.reshape(-1, D_MODEL)
    scores = jax.nn.sigmoid((t @ w_router).astype(jnp.float32))
    biased = scores + router_bias.astype(jnp.float32)
    grp = biased.reshape(-1, N_GROUPS, N_EXPERTS // N_GROUPS)
    grp_score = jnp.sum(lax.top_k(grp, 2)[0], axis=-1)
    _, gidx = lax.top_k(grp_score, TOPK_GROUPS)
    gmask = jnp.sum(jax.nn.one_hot(gidx, N_GROUPS, dtype=jnp.float32), axis=1)
    emask = jnp.repeat(gmask, N_EXPERTS // N_GROUPS, axis=1)
    masked = jnp.where(emask > 0, biased, -jnp.inf)
    _, eidx = lax.top_k(masked, TOP_K)
    w = jnp.take_along_axis(scores, eidx, axis=-1)
    w = w / jnp.sum(w, axis=-1, keepdims=True) * ROUTED_SCALE
    gates = jnp.sum(jax.nn.one_hot(eidx, N_EXPERTS, dtype=jnp.float32) * w[..., None], axis=1)

    tb = t.reshape(-1, MOE_BLOCK, D_MODEL)
    gb = gates.reshape(-1, MOE_BLOCK, N_EXPERTS)

    def block(args):
        xb, g = args
        a = jnp.einsum('td,edf->tef', xb, we_gate)
        b = jnp.einsum('td,edf->tef', xb, we_up)
        hm = jax.nn.silu(a) * b * g[..., None].astype(xb.dtype)
        return jnp.einsum('tef,efd->td', hm, we_down)

    routed = lax.map(block, (tb, gb)).reshape(t.shape)
    shared = (jax.nn.silu(t @ ws_gate) * (t @ ws_up)) @ ws_down
    return (routed + shared).reshape(shape)


def setup_inputs(seed: int = 0) -> dict:
    key = jax.random.key(seed)
    ks = jax.random.split(key, 32)
    D = D_MODEL

    def nrm(k, shape, scale):
        return jax.random.normal(k, shape, jnp.float32) * scale

    return {
        "x": nrm(ks[0], (BATCH, SEQ, D), 1.0),
        "c": nrm(ks[1], (BATCH, D), 1.0),
        "ctx": nrm(ks[2], (BATCH, CTX_LEN, D), 1.0),
        "c_ctx": nrm(ks[3], (D,), 1.0),
        "w_ada": nrm(ks[4], (DEPTH, D, 6 * D), 0.5 * D ** -0.5),
        "b_ada": nrm(ks[5], (DEPTH, 6 * D), 0.02),
        "norm_mix_g": 1.0 + nrm(ks[6], (DEPTH, D), 0.05),
        "w_in": nrm(ks[7], (DEPTH, D, IN_WIDTH), D ** -0.5),
        "q_norm_g": 1.0 + nrm(ks[8], (DEPTH, DA_HEAD_DIM), 0.05),
        "k_norm_g": 1.0 + nrm(ks[9], (DEPTH, DA_HEAD_DIM), 0.05),
        "da_lambda": nrm(ks[10], (DEPTH, 4, DA_HEAD_DIM), 0.1),
        "subln_g": 1.0 + nrm(ks[11], (DEPTH, DA_V_DIM), 0.05),
        "gm_ln_g": 1.0 + nrm(ks[12], (DEPTH, GM_HEADS, GM_HEAD_DIM), 0.05),
        "gm_ln_b": nrm(ks[13], (DEPTH, GM_HEADS, GM_HEAD_DIM), 0.02),
        "gm_ws": nrm(ks[14], (DEPTH, GM_HEADS, CHUNK, CHUNK), CHUNK ** -0.5),
        "gm_bs": 1.0 + nrm(ks[15], (DEPTH, GM_HEADS, CHUNK), 0.1),
        "gm_out_g": 1.0 + nrm(ks[16], (DEPTH, GM_HEADS, GM_HEAD_DIM), 0.05),
        "w_out": nrm(ks[17], (DEPTH, MIX_WIDTH, D), MIX_WIDTH ** -0.5),
        "norm_ffn_g": 1.0 + nrm(ks[18], (DEPTH, D), 0.05),
        "w_router": nrm(ks[19], (DEPTH, D, N_EXPERTS), D ** -0.5),
        "router_bias": nrm(ks[20], (DEPTH, N_EXPERTS), 0.01),
        "we_gate": nrm(ks[21], (DEPTH, N_EXPERTS, D, D_EXPERT), D ** -0.5),
        "we_up": nrm(ks[22], (DEPTH, N_EXPERTS, D, D_EXPERT), D ** -0.5),
        "we_down": nrm(ks[23], (DEPTH, N_EXPERTS, D_EXPERT, D), D_EXPERT ** -0.5),
        "ws_gate": nrm(ks[24], (DEPTH, D, D_SHARED), D ** -0.5),
        "ws_up": nrm(ks[25], (DEPTH, D, D_SHARED), D ** -0.5),
        "ws_down": nrm(ks[26], (DEPTH, D_SHARED, D), D_SHARED ** -0.5),
    }


def reference(x, c, ctx, c_ctx, w_ada, b_ada, norm_mix_g, w_in, q_norm_g, k_norm_g, da_lambda,
              subln_g, gm_ln_g, gm_ln_b, gm_ws, gm_bs, gm_out_g, w_out, norm_ffn_g, w_router,
              router_bias, we_gate, we_up, we_down, ws_gate, ws_up, ws_down):
    B, L, _ = x.shape
    cos, sin = axial_rope_tables(L)
    nb = L // Q_BLOCK

    for l in range(DEPTH):
        lambda_init = 0.8 - 0.6 * math.exp(-0.3 * l)
        lp = da_lambda[l].astype(jnp.float32)
        lam = jnp.exp(jnp.sum(lp[0] * lp[1])) - jnp.exp(jnp.sum(lp[2] * lp[3])) + lambda_init

        sh_m, sc_m, g_m, sh_f, sc_f, g_f = [m[:, None, :] for m in adaln_params(c, w_ada[l], b_ada[l])]
        csh_m, csc_m, cg_m, csh_f, csc_f, cg_f = adaln_params(c_ctx, w_ada[l], b_ada[l])

        hx = modulate(rms_norm(x, norm_mix_g[l]), sh_m, sc_m)
        hc = modulate(rms_norm(ctx, norm_mix_g[l]), csh_m, csc_m)
        qx, kx, vx, ux, gx = split_proj(hx, w_in[l])
        qc, kc, vc, uc, gc = split_proj(hc, w_in[l])
        qx = apply_rope(rms_norm(qx, q_norm_g[l]), cos, sin)
        kx = apply_rope(rms_norm(kx, k_norm_g[l]), cos, sin)
        qc = rms_norm(qc, q_norm_g[l])
        kc = rms_norm(kc, k_norm_g[l])

        k_all = jnp.concatenate([kc, kx], axis=3)
        v_all = jnp.concatenate([vc, vx], axis=2)
        q_blocks = jnp.moveaxis(qx.reshape(B, DA_HEADS, 2, nb, Q_BLOCK, DA_HEAD_DIM), 3, 0)
        o = lax.map(lambda qb: diff_attend(qb, k_all, v_all, lam), q_blocks)
        o = jnp.moveaxis(o, 0, 2).reshape(B, DA_HEADS, L, DA_V_DIM)
        attn_x = da_output(o, subln_g[l], lambda_init)
        gm_x = chunk_gmlp(ux, gx, gm_ln_g[l], gm_ln_b[l], gm_ws[l], gm_bs[l], gm_out_g[l])
        mix_x = jnp.concatenate([attn_x, gm_x], axis=-1) @ w_out[l]

        if l < DEPTH - 1:
            attn_c = da_output(diff_attend(qc, kc, vc, lam), subln_g[l], lambda_init)
            gm_c = chunk_gmlp(uc, gc, gm_ln_g[l], gm_ln_b[l], gm_ws[l], gm_bs[l], gm_out_g[l])
            mix_c = jnp.concatenate([attn_c, gm_c], axis=-1) @ w_out[l]
            ctx = ctx + cg_m * mix_c

        x = x + g_m * mix_x

        fx = modulate(rms_norm(x, norm_ffn_g[l]), sh_f, sc_f)
        x = x + g_f * moe_ffn(fx, w_router[l], router_bias[l], we_gate[l], we_up[l], we_down[l],
                              ws_gate[l], ws_up[l], ws_down[l])
        if l < DEPTH - 1:
            fc = modulate(rms_norm(ctx, norm_ffn_g[l]), csh_f, csc_f)
            ctx = ctx + cg_f * moe_ffn(fc, w_router[l], router_bias[l], we_gate[l], we_up[l],
                                       we_down[l], ws_gate[l], ws_up[l], ws_down[l])
    return x
```

```python
import math
from contextlib import ExitStack

import numpy as np
import concourse.bass as bass
import concourse.mybir as mybir
from concourse.bass_utils import run_bass_kernel_spmd

F32 = mybir.dt.float32
BF16 = mybir.dt.bfloat16
AF = mybir.ActivationFunctionType
ALU = mybir.AluOpType
AX = mybir.AxisListType

D = 1024
L = 4096
CTX = 256
NT = L // 128
NKC = (CTX + L) // 128
EPS = 1e-6
LAMBDA_INIT = 0.8 - 0.6 * math.exp(-0.3 * 0)
NEXP = 32

ENGS = ["pe", "act", "dve", "pool", "sp"]


class Op:
    __slots__ = ("eng", "fn", "deps", "need_inc", "sem", "val", "is_dma", "chan", "emitted")


class Sched:
    def __init__(self, nc, stack):
        self.nc = nc
        self.stack = stack
        self.pending = []
        self.last_w = {}
        self.readers = {}
        self.chan_last = {}
        self.chan_count = {}
        self.cnt = {e: 0 for e in ENGS}
        self.esem = {e: stack.enter_context(nc.semaphore("s_" + e)) for e in ENGS}
        self.csem = {}
        self.known = {e: {} for e in ENGS}
        self.last_op = {}

    def add(self, eng, fn, r=(), w=(), chan=None, extra=()):
        op = Op()
        op.eng = eng
        op.fn = fn
        op.is_dma = chan is not None
        op.chan = chan
        op.need_inc = op.is_dma
        op.sem = None
        op.val = None
        op.emitted = False
        deps = set(extra)
        for b in r:
            o = self.last_w.get(b)
            if o is not None:
                deps.add(o)
        for b in w:
            o = self.last_w.get(b)
            if o is not None:
                deps.add(o)
            for o in self.readers.get(b, ()):
                deps.add(o)
        if op.is_dma and chan in self.chan_last:
            deps.add(self.chan_last[chan])
        deps.discard(op)
        for b in r:
            self.readers.setdefault(b, []).append(op)
        for b in w:
            self.last_w[b] = op
            self.readers[b] = []
        if op.is_dma:
            self.chan_last[chan] = op
            self.chan_count[chan] = self.chan_count.get(chan, 0) + 1
            op.val = 16 * self.chan_count[chan]
            if chan not in self.csem:
                self.csem[chan] = self.stack.enter_context(self.nc.semaphore("c_%d" % len(self.csem)))
            op.sem = self.csem[chan]
        else:
            op.sem = self.esem[eng]
        op.deps = deps
        self.pending.append(op)
        self.last_op[eng] = op
        return op

    @staticmethod
    def _skip(d, op):
        if d.is_dma:
            return False
        if d.emitted:
            return True
        return (not op.is_dma) and d.eng == op.eng and d.eng == "pe"

    def emit(self):
        self.add("sp", lambda e: e.nop(), extra=list(self.chan_last.values()))
        ops = self.pending
        self.pending = []
        for op in ops:
            for d in op.deps:
                if d.is_dma or self._skip(d, op):
                    continue
                d.need_inc = True
        for op in ops:
            if not op.is_dma and op.need_inc:
                self.cnt[op.eng] += 1
                op.val = self.cnt[op.eng]
        per = {e: [o for o in ops if o.eng == e] for e in ENGS}
        sched = self

        def run(name, eng):
            known = sched.known[name]
            for op in per[name]:
                waits = {}
                for d in op.deps:
                    if sched._skip(d, op):
                        continue
                    key = id(d.sem)
                    if known.get(key, 0) >= d.val:
                        continue
                    if key not in waits or waits[key][1] < d.val:
                        waits[key] = (d.sem, d.val)
                for key, (sem, val) in waits.items():
                    eng.wait_ge(sem, val)
                    known[key] = val
                inst = op.fn(eng)
                if op.need_inc:
                    inst.then_inc(op.sem, 16 if op.is_dma else 1)

        with self.nc.Block(no_gpsimd_drain=True) as block:
            @block.tensor
            def _(e):
                run("pe", e)

            @block.scalar
            def _(e):
                run("act", e)

            @block.vector
            def _(e):
                run("dve", e)

            @block.gpsimd
            def _(e):
                run("pool", e)

            @block.sync
            def _(e):
                run("sp", e)
        for op in ops:
            op.emitted = True

    def keep(self, op):
        op.need_inc = True
        return op


def build_program(debug=False):
    nc = bass.Bass("TRN2", target_bir_lowering=False)

    def din(name, shape):
        return nc.dram_tensor(name, list(shape), F32, kind="ExternalInput").ap()

    x_d = din("x", [L, D])
    ctx_d = din("ctx", [CTX, D])
    cc_d = din("cc", [128, 16])
    wada_d = din("w_ada", [D, 6 * D])
    bada_d = din("b_adaT", [128, 48])
    nmg_d = din("nmg", [128, 8])
    nfg_d = din("nfg", [128, 8])
    win_d = din("w_in", [D, 2560])
    qkg_d = din("qkg", [128, 128])
    lam_d = din("lam_in", [128, 256])
    subg_d = din("subg", [128, 128])
    gml_d = din("gml", [128, 3 * 512])
    wsT_d = din("w_sT", [128, 512])
    bsT_d = din("bsT", [128, 4])
    wout_d = din("w_out", [D, D])
    wr_d = din("w_router", [D, 32])
    rb_d = din("rbias", [128, 32])
    weg_d = din("we_gate", [NEXP, D, 256])
    weu_d = din("we_up", [NEXP, D, 256])
    wed_d = din("we_down", [NEXP, 256, D])
    wsg_d = din("ws_gate", [D, 256])
    wsu_d = din("ws_up", [D, 256])
    wsd_d = din("ws_down", [256, D])
    cos_d = din("cos", [L, 32])
    sin_d = din("sin", [L, 32])
    ident_d = din("ident", [128, 128])
    out_d = nc.dram_tensor("out", [L, D], F32, kind="ExternalOutput").ap()
    x1_d = nc.dram_tensor("x1s", [L, D], F32, kind="Internal").ap()
    dbg = {}
    if debug:
        for nm, shp in [("d_mod", [128, 96]), ("d_kt", [128, 4 * 4352]), ("d_v1", [128, 34 * 4 * 130]),
                        ("d_gmt", [128, 4 * 4096]), ("d_gates", [128, 32 * 32])]:
            dbg[nm] = nc.dram_tensor(nm, shp, F32, kind="ExternalOutput").ap()

    with ExitStack() as g:
        S = Sched(nc, g)
        A = S.add

        def sb(name, shape, dt=F32, st=None):
            return (st or g).enter_context(nc.sbuf_tensor("sb_" + name, list(shape), dt))

        PS2 = [g.enter_context(nc.psum_tensor("psp%d" % i, [128, 1024], F32)) for i in range(2)]
        PS2B = [p.bitcast(BF16) for p in PS2]
        PS = [PS2[i // 2][:, (i % 2) * 512:(i % 2 + 1) * 512] for i in range(4)]
        PSB = [PS2B[i // 2][:, (i % 2) * 1024:(i % 2 + 1) * 1024] for i in range(4)]
        _ps47 = [g.enter_context(nc.psum_tensor("ps%d" % i, [128, 512], F32)) for i in range(4, 8)]
        PS = PS + _ps47
        PSB = PSB + [p.bitcast(BF16) for p in _ps47]

        def pk(i):
            return ("ps", i)

        identf = sb("identf", [128, 128])
        identb = sb("identb", [128, 128], BF16)
        onesf = sb("onesf", [128, 128])
        mhalf = sb("mhalf", [128, 16])
        cc = sb("cc", [128, 16])
        scs = sb("scs", [128, 16])
        badaT = sb("badaT", [128, 48])
        mod = sb("mod", [128, 96])
        mod3 = mod[:].rearrange("p (c j) -> p c j", j=2)
        nmg = sb("nmg", [128, 8])
        nfg = sb("nfg", [128, 8])
        smix = sb("smix", [128, 16])
        smix3 = smix[:].rearrange("p (c j) -> p c j", j=2)
        sffn = sb("sffn", [128, 8])
        qkg = sb("qkg", [128, 128])
        lsm = sb("lsm", [128, 8])
        bsT = sb("bsT", [128, 4])
        rbias = sb("rbias", [128, 32])
        wr = sb("wr", [128, 8, 32], BF16)
        dg = sb("dg", [128, 128])
        junk = sb("junk", [128, 1024], BF16)
        junk2 = sb("junk2", [128, 512], BF16)
        junk3 = sb("junk3", [128, 1024], BF16)

        def load(eng, name, dst, src, key=None):
            return A(eng, lambda e: e.dma_start(out=dst, in_=src), w=[key or name], chan=name)

        load("sp", "identf", identf[:], ident_d)
        load("sp", "cc", cc[:], cc_d)
        load("sp", "badaT", badaT[:], bada_d)
        load("sp", "nmg", nmg[:], nmg_d)
        load("sp", "nfg", nfg[:], nfg_d)
        load("sp", "qkg", qkg[:], qkg_d)
        load("sp", "bsT", bsT[:], bsT_d)
        load("sp", "rbias", rbias[:], rb_d)
        load("pool", "wr", wr[:], wr_d.rearrange("(k p) n -> p k n", p=128))
        A("dve", lambda e: e.tensor_copy(out=identb[:], in_=identf[:]), r=["identf"], w=["identb"])
        A("dve", lambda e: e.memset(onesf[:], 1.0), w=["onesf"])
        A("dve", lambda e: e.memset(mhalf[:], -0.5), w=["mhalf"])

        def rsq(out, in_, n, mul, add, tmp, rkeys, wkey):
            A("dve", lambda e: e.tensor_scalar(out=tmp, in0=in_, scalar1=mul, scalar2=add, op0=ALU.mult, op1=ALU.add),
              r=rkeys, w=[wkey + "_t"])
            A("pool", lambda e: e.tensor_tensor(out=out, in0=tmp, in1=mhalf[:, 0:n], op=ALU.pow),
              r=[wkey + "_t", "mhalf"], w=[wkey])

        def round_robin(gens):
            gens = list(gens)
            while gens:
                for g_ in list(gens):
                    try:
                        next(g_)
                    except StopIteration:
                        gens.remove(g_)

        def bcast_rows(dst, base, key):
            for half in range(2):
                for kk in range(4):
                    k = half * 4 + kk
                    A("dve", lambda e, k=k: e.tensor_scalar(out=dg[:], in0=identf[:], scalar1=mod3[:, base + k, 0:1],
                                                            scalar2=None, op0=ALU.mult),
                      r=["identf", "mod"], w=["dg"])
                    A("pe", lambda e, kk=kk: e.matmul(PS[1][:, kk * 128:(kk + 1) * 128], lhsT=onesf[:], rhs=dg[:],
                                                      start=True, stop=True), r=["onesf", "dg"], w=[pk(1)])
                A("dve", lambda e, half=half: e.tensor_copy(out=dst[:, half * 512:(half + 1) * 512], in_=PS[1][:]),
                  r=[pk(1)], w=[key])

        with ExitStack() as pa:
            lam_in = sb("lam_in", [128, 256], F32, pa)
            load("sp", "lam_in", lam_in[:], lam_d)
            wa = [sb("wa%d" % i, [128, 8, 512], F32, pa) for i in range(2)]
            A("act", lambda e: e.activation(out=scs[:], in_=cc[:], func=AF.Silu), r=["cc"], w=["scs"])
            for c in range(12):
                bf = c % 2
                A("sp", lambda e, c=c, bf=bf: e.dma_start(
                    out=wa[bf][:], in_=wada_d[:, c * 512:(c + 1) * 512].rearrange("(k p) n -> p k n", p=128)),
                  w=[("wa", bf)], chan=("wa", bf))
                for j in range(4):
                    col = c * 4 + j
                    for k in range(8):
                        A("pe", lambda e, bf=bf, j=j, k=k, col=col: e.matmul(
                            PS[0][:, col * 2:col * 2 + 2], lhsT=wa[bf][:, k, j * 128:(j + 1) * 128],
                            rhs=scs[:, 2 * k:2 * k + 2], start=(k == 0), stop=(k == 7)),
                          r=[("wa", bf), "scs"], w=[pk(0)])
            ps0v = PS[0][:, 0:96].rearrange("p (c j) -> p c j", j=2)
            for j in range(2):
                A("dve", lambda e, j=j: e.tensor_tensor(out=mod3[:, :, j], in0=ps0v[:, :, j], in1=badaT[:], op=ALU.add),
                  r=[pk(0), "badaT"], w=["mod"])
            for j in range(2):
                A("dve", lambda e, j=j: e.scalar_tensor_tensor(out=smix3[:, :, j], in0=mod3[:, 8:16, j], scalar=1.0,
                                                                in1=nmg[:], op0=ALU.add, op1=ALU.mult),
                  r=["mod", "nmg"], w=["smix"])
            A("dve", lambda e: e.scalar_tensor_tensor(out=sffn[:], in0=mod3[:, 32:40, 0], scalar=1.0, in1=nfg[:],
                                                      op0=ALU.add, op1=ALU.mult), r=["mod", "nfg"], w=["sffn"])
            for i in range(2):
                A("dve", lambda e, i=i: e.scalar_tensor_tensor(
                    out=junk2[:, 0:64], in0=lam_in[:, i * 128:i * 128 + 64], scalar=1.0,
                    in1=lam_in[:, i * 128 + 64:i * 128 + 128], op0=ALU.mult, op1=ALU.mult, accum_out=lsm[:, i:i + 1]),
                  r=["lam_in"], w=["junk2", ("lsm", i)])
            A("act", lambda e: e.activation(out=lsm[:, 2:4], in_=lsm[:, 0:2], func=AF.Exp), r=[("lsm", 0), ("lsm", 1)], w=["lse"])
            A("dve", lambda e: e.tensor_tensor(out=lsm[:, 4:5], in0=lsm[:, 3:4], in1=lsm[:, 2:3], op=ALU.subtract),
              r=["lse"], w=["nl0"])
            A("dve", lambda e: e.tensor_scalar(out=lsm[:, 5:6], in0=lsm[:, 4:5], scalar1=-LAMBDA_INIT, scalar2=None, op0=ALU.add),
              r=["nl0"], w=["nlam"])
            nlam = lsm[:, 5:6]
            if debug:
                dm = A("sp", lambda e: e.dma_start(out=dbg["d_mod"], in_=mod[:]), r=["mod"], chan="d_mod")
            S.emit()

        with ExitStack() as pb:
            KT = sb("KT", [128, 4, NKC * 128], BF16, pb)
            V1 = sb("V1", [128, NKC, 4, 130], BF16, pb)
            gmT = sb("gmT", [128, 4, L], BF16, pb)
            A("pool", lambda e: e.memset(V1[:].rearrange("p a b c -> p (a b c)"), 1.0), w=["V1init"])

            def xnorm(xt, xkey, ssq, rstd, tmp1, xn, tag, on_dve=False):
                if on_dve:
                    A("dve", lambda e: e.scalar_tensor_tensor(out=junk3[:], in0=xt, scalar=1.0, in1=xt, op0=ALU.mult, op1=ALU.mult,
                                                              accum_out=ssq),
                      r=[xkey], w=["junk3", tag + "ssq"])
                else:
                    A("act", lambda e: e.activation(out=junk[:], in_=xt, func=AF.Square, accum_out=ssq),
                      r=[xkey], w=["junk", tag + "ssq"])
                rsq(rstd, ssq, 1, 1.0 / D, EPS, tmp1, [tag + "ssq"], tag + "rstd")
                A("dve", lambda e: e.tensor_scalar(out=xn, in0=xt, scalar1=rstd, scalar2=None, op0=ALU.mult),
                  r=[xkey, tag + "rstd"], w=[tag + "xn"])

            def fm_transposes(xn, xnkey, bank):
                for k in range(8):
                    A("pe", lambda e, k=k: e.transpose(out=PSB[bank][:, k * 128:(k + 1) * 128],
                                                       in_=xn[:, k * 128:(k + 1) * 128], identity=identb[:]),
                      r=[xnkey, "identb"], w=[pk(bank)])

            def to_featmajor(xn, xnkey, bank, scale3, bias3, j, hxT, hkey, do_transposes=True, dve_ks=()):
                if do_transposes:
                    fm_transposes(xn, xnkey, bank)
                for k in range(8):
                    if k in dve_ks:
                        A("dve", lambda e, k=k: e.tensor_scalar(out=hxT[:, k, :], in0=PSB[bank][:, k * 128:(k + 1) * 128],
                                                                scalar1=scale3(k, j), scalar2=bias3(k, j), op0=ALU.mult, op1=ALU.add),
                          r=[pk(bank), "smix", "mod", "sffn"], w=[hkey])
                    else:
                        A("act", lambda e, k=k: e.activation(out=hxT[:, k, :], in_=PSB[bank][:, k * 128:(k + 1) * 128],
                                                             func=AF.Identity, scale=scale3(k, j), bias=bias3(k, j)),
                          r=[pk(bank), "smix", "mod", "sffn"], w=[hkey])

            def qknorm_rope_gen(src, srckey, goff, rope, cos_t, sin_t, ckeys, T, outb, okey, tag, engs=("pool",) * 5):
                t0, t1, t2, st8, st8b, st8t = T
                pv = src.rearrange("p (g d) -> p g d", g=8)
                t1v = t1.rearrange("p (g d) -> p g d", g=8)
                t1k = tag + "t1"
                A("act", lambda e: e.activation(out=t0, in_=src, func=AF.Square), r=[srckey], w=[tag + "t0"])
                yield
                A("dve", lambda e: e.tensor_reduce(out=st8, in_=t0.rearrange("p (g d) -> p g d", g=8), axis=AX.X, op=ALU.add),
                  r=[tag + "t0"], w=[tag + "st8"])
                A("dve", lambda e: e.tensor_scalar(out=st8t, in0=st8, scalar1=1.0 / 64, scalar2=EPS, op0=ALU.mult, op1=ALU.add),
                  r=[tag + "st8"], w=[tag + "st8b_t"])
                yield
                A("pool", lambda e: e.tensor_tensor(out=st8b, in0=st8t, in1=mhalf[:, 0:8], op=ALU.pow),
                  r=[tag + "st8b_t", "mhalf"], w=[tag + "st8b"])
                yield
                A("dve", lambda e: e.tensor_tensor(out=t1v, in0=pv, in1=st8b.unsqueeze(2).to_broadcast([128, 8, 64]), op=ALU.mult),
                  r=[srckey, tag + "st8b"], w=[t1k])
                gain = qkg[:, goff:goff + 64].unsqueeze(1).to_broadcast([128, 8, 64])
                if not rope:
                    A("dve", lambda e: e.tensor_tensor(out=outb.rearrange("p (g d) -> p g d", g=8), in0=t1v, in1=gain, op=ALU.mult),
                      r=[t1k, "qkg"], w=[okey])
                    yield
                    return
                yield
                A(engs[0], lambda e: e.tensor_tensor(out=t1v, in0=t1v, in1=gain, op=ALU.mult), r=[t1k, "qkg"], w=[t1k])
                x1 = t1v[:, :, 0::2]
                x2 = t1v[:, :, 1::2]
                cb = cos_t.unsqueeze(1).to_broadcast([128, 8, 32])
                sbb = sin_t.unsqueeze(1).to_broadcast([128, 8, 32])
                t2v = t2.rearrange("p (i g d) -> p i g d", i=4, g=8)
                ov = outb.rearrange("p (g d) -> p g d", g=8)
                A(engs[1], lambda e: e.tensor_tensor(out=t2v[:, 0], in0=x1, in1=cb, op=ALU.mult), r=[t1k] + ckeys, w=[tag + "ra"])
                A(engs[2], lambda e: e.tensor_tensor(out=t2v[:, 1], in0=x2, in1=sbb, op=ALU.mult), r=[t1k] + ckeys, w=[tag + "rb"])
                yield
                A("dve", lambda e: e.tensor_tensor(out=ov[:, :, 0::2], in0=t2v[:, 0], in1=t2v[:, 1], op=ALU.subtract),
                  r=[tag + "ra", tag + "rb"], w=[okey])
                A(engs[3], lambda e: e.tensor_tensor(out=t2v[:, 2], in0=x1, in1=sbb, op=ALU.mult), r=[t1k] + ckeys, w=[tag + "rc"])
                A(engs[4], lambda e: e.tensor_tensor(out=t2v[:, 3], in0=x2, in1=cb, op=ALU.mult), r=[t1k] + ckeys, w=[tag + "rd"])
                yield
                A("dve", lambda e: e.tensor_tensor(out=ov[:, :, 1::2], in0=t2v[:, 2], in1=t2v[:, 3], op=ALU.add),
                  r=[tag + "rc", tag + "rd", okey], w=[okey])
                yield

            with ExitStack() as p1:
                wkv = sb("wkv", [128, 8, 2048], BF16, p1)
                gml = sb("gml", [128, 1536], F32, p1)
                wsT = sb("wsT", [128, 512], BF16, p1)
                load("sp", "gml", gml[:], gml_d)
                load("pool", "wsT", wsT[:], wsT_d)
                for i in range(4):
                    A("pool", lambda e, i=i: e.dma_start(
                        out=wkv[:, :, i * 512:(i + 1) * 512],
                        in_=win_d[:, 512 + i * 512:512 + (i + 1) * 512].rearrange("(k p) n -> p k n", p=128)),
                      w=[("wkv", i)], chan=("wkv", i))
                xt = [sb("xt%d" % i, [128, 1024], F32, p1) for i in range(2)]
                cst = [sb("cst%d" % i, [128, 64], F32, p1) for i in range(2)]
                xn = sb("xn", [128, 1024], BF16, p1)
                hxT = [sb("hxT%d" % i, [128, 8, 128], BF16, p1) for i in range(2)]
                st = sb("st", [128, 64], F32, p1)
                t0 = sb("t0", [128, 512], F32, p1)
                t1 = sb("t1", [128, 512], F32, p1)
                t2 = sb("t2", [128, 1024], F32, p1)
                kr = sb("kr", [128, 512], BF16, p1)
                us = sb("us", [128, 512], F32, p1)
                gs = sb("gs", [128, 512], F32, p1)
                vc = sb("vc", [128, 512], F32, p1)
                vn = sb("vn", [128, 512], BF16, p1)
                yy = sb("yy", [128, 512], F32, p1)
                ysq = sb("ysq", [128, 512], F32, p1)
                gmx = sb("gmx", [128, 512], BF16, p1)
                bst = sb("bst", [128, 4, 6], F32, p1)
                mv = sb("mv", [128, 4, 2], F32, p1)

                kraw = sb("kraw", [128, 512], F32, p1)
                kraw2 = sb("kraw2", [128, 512], F32, p1)
                vn2 = sb("vn2", [128, 512], BF16, p1)
                kr2 = sb("kr2", [128, 512], BF16, p1)
                krs = [kr, kr2]
                vns = [vn, vn2]
                kraws = [kraw, kraw2]
                t1b = sb("t1b", [128, 512], F32, p1)
                usb = sb("usb", [128, 512], F32, p1)
                gsb = sb("gsb", [128, 512], F32, p1)
                cst3 = sb("cst3", [128, 64], F32, p1)
                csts = [cst[0], cst[1], cst3]
                t1s = [t1, t1b]
                uss = [us, usb]
                gss = [gs, gsb]

                def b1_load_x(t):
                    bf = t % 2
                    src = ctx_d[t * 128:(t + 1) * 128, :] if t < 2 else x_d[(t - 2) * 128:(t - 1) * 128, :]
                    A("sp", lambda e: e.dma_start(out=xt[bf][:], in_=src), w=[("xt", bf)], chan=("xt", bf))

                def b1_load_cs(t):
                    cb3 = t % 3
                    if t >= 2:
                        r0 = (t - 2) * 128
                        A("sp", lambda e: e.dma_start(out=csts[cb3][:, 0:32], in_=cos_d[r0:r0 + 128, :]), w=[("cs", cb3, 0)], chan=("cs", cb3, 0))
                        A("sp", lambda e: e.dma_start(out=csts[cb3][:, 32:64], in_=sin_d[r0:r0 + 128, :]), w=[("cs", cb3, 1)], chan=("cs", cb3, 1))

                def Fa1_gen(t):
                    bf = t % 2
                    xnorm(xt[bf][:], ("xt", bf), st[:, 0:1], st[:, 1:2], st[:, 2:3], xn[:], "b1")
                    yield

                def Fa2_gen(t):
                    bf = t % 2
                    j = 1 if t < 2 else 0
                    fm_transposes(xn, "b1xn", 0)
                    yield
                    yield
                    to_featmajor(xn, "b1xn", 0, lambda k, j: smix3[:, k, j:j + 1], lambda k, j: mod3[:, k, j:j + 1], j,
                                 hxT[bf], ("hxT", bf), do_transposes=False)
                    yield

                def Fb_gen(t):
                    bf = t % 2
                    nproj = 2 if t < 2 else 4
                    for i in range(nproj):
                        for k in range(8):
                            A("pe", lambda e, i=i, k=k: e.matmul(PS[1 + i][:], lhsT=hxT[bf][:, k, :],
                                                                 rhs=wkv[:, k, i * 512:(i + 1) * 512],
                                                                 start=(k == 0), stop=(k == 7)),
                              r=[("hxT", bf), ("wkv", i)], w=[pk(1 + i)])
                        yield

                def K_gen(t):
                    par = t % 2
                    cb3 = t % 3
                    kraw_ = kraws[par]
                    A("act", lambda e: e.activation(out=kraw_[:], in_=PS[1][:], func=AF.Copy), r=[pk(1)], w=[("kraw", par)])
                    A("act", lambda e: e.activation(out=V1[:, t, :, 0:128],
                                                    in_=PS[2][:].rearrange("p (h d) -> p h d", h=4), func=AF.Copy),
                      r=[pk(2), "V1init"], w=[("V1", t)])
                    yield
                    yield from qknorm_rope_gen(kraw_[:], ("kraw", par), 64, t >= 2, csts[cb3][:, 0:32], csts[cb3][:, 32:64],
                                               [("cs", cb3, 0), ("cs", cb3, 1)],
                                               (t0[:], t1s[par][:], t2[:], st[:, 8:16], st[:, 16:24], st[:, 24:32]),
                                               krs[par][:], ("kr", par), "k%d" % par, engs=("pool", "pool", "dve", "pool", "dve"))

                def Kt_gen(t):
                    kr_ = krs[t % 2]
                    for h in range(4):
                        A("pe", lambda e, h=h: e.transpose(out=PSB[5][:, h * 128:(h + 1) * 128],
                                                           in_=kr_[:, h * 128:(h + 1) * 128], identity=identb[:]),
                          r=[("kr", t % 2), "identb"], w=[pk(5)])
                    yield
                    yield
                    A("act", lambda e: e.activation(out=KT[:, :, t * 128:(t + 1) * 128],
                                                    in_=PSB[5][:, 0:512].rearrange("p (h t) -> p h t", h=4), func=AF.Copy),
                      r=[pk(5)], w=[("KT", t)])
                    yield

                def G_gen(t):
                    par = t % 2
                    us_, gs_ = uss[par], gss[par]
                    uk, gk = ("us", par), ("gs", par)
                    A("act", lambda e: e.activation(out=us_[:], in_=PS[3][:], func=AF.Gelu), r=[pk(3)], w=[uk])
                    A("act", lambda e: e.activation(out=gs_[:], in_=PS[4][:], func=AF.Gelu), r=[pk(4)], w=[gk])
                    yield
                    for h in range(4):
                        A("dve", lambda e, h=h: e.bn_stats(out=bst[:, h, :], in_=gs_[:, h * 128:(h + 1) * 128]), r=[gk], w=[("bst", h)])
                    for h in range(4):
                        A("dve", lambda e, h=h: e.bn_aggr(out=mv[:, h, :], in_=bst[:, h, :]), r=[("bst", h)], w=[("mv", h)])
                    A("dve", lambda e: e.tensor_scalar(out=st[:, 36:40], in0=mv[:, :, 1], scalar1=1.0, scalar2=EPS, op0=ALU.mult, op1=ALU.add),
                      r=[("mv", h) for h in range(4)], w=["lnr_t"])
                    yield
                    A("pool", lambda e: e.tensor_tensor(out=st[:, 32:36], in0=st[:, 36:40], in1=mhalf[:, 0:4], op=ALU.pow),
                      r=["lnr_t", "mhalf"], w=["lnr"])
                    yield
                    for h in range(4):
                        A("dve", lambda e, h=h: e.tensor_scalar(out=vc[:, h * 128:(h + 1) * 128], in0=gs_[:, h * 128:(h + 1) * 128],
                                                                scalar1=mv[:, h, 0:1], scalar2=st[:, 32 + h:33 + h],
                                                                op0=ALU.subtract, op1=ALU.mult),
                          r=[gk, ("mv", h), "lnr"], w=[("vc", h)])
                    yield
                    A("pool", lambda e: e.tensor_tensor(out=vc[:], in0=vc[:], in1=gml[:, 0:512], op=ALU.mult),
                      r=[("vc", h) for h in range(4)] + ["gml"], w=[("vc", h) for h in range(4)])
                    vn_ = vns[par]
                    A("pool", lambda e: e.tensor_tensor(out=vn_[:], in0=vc[:], in1=gml[:, 512:1024], op=ALU.add),
                      r=[("vc", h) for h in range(4)] + ["gml"], w=[("vn", par)])
                    yield

                def G2_gen(t):
                    par = t % 2
                    us_ = uss[par]
                    uk = ("us", par)
                    vn_ = vns[par]
                    for h in range(4):
                        A("pe", lambda e, h=h: e.matmul(PS[6][:, h * 128:(h + 1) * 128], lhsT=wsT[:, h * 128:(h + 1) * 128],
                                                        rhs=vn_[:, h * 128:(h + 1) * 128], start=True, stop=True),
                          r=["wsT", ("vn", par)], w=[pk(6)])
                    yield
                    yield
                    for h in range(4):
                        A("dve", lambda e, h=h: e.scalar_tensor_tensor(
                            out=yy[:, h * 128:(h + 1) * 128], in0=PS[6][:, h * 128:(h + 1) * 128], scalar=bsT[:, h:h + 1],
                            in1=us_[:, h * 128:(h + 1) * 128], op0=ALU.add, op1=ALU.mult),
                          r=[pk(6), "bsT", uk], w=[("yy", h)])
                    yield
                    for h in range(4):
                        A("dve", lambda e, h=h: e.scalar_tensor_tensor(
                            out=ysq[:, h * 128:(h + 1) * 128], in0=yy[:, h * 128:(h + 1) * 128], scalar=1.0,
                            in1=yy[:, h * 128:(h + 1) * 128], op0=ALU.mult, op1=ALU.mult, accum_out=st[:, 40 + h:41 + h]),
                          r=[("yy", h)], w=[("ysq", h), ("yss", h)])
                    A("dve", lambda e: e.tensor_scalar(out=st[:, 48:52], in0=st[:, 40:44], scalar1=1.0 / 128, scalar2=EPS, op0=ALU.mult, op1=ALU.add),
                      r=[("yss", h) for h in range(4)], w=["yr_t"])
                    yield
                    A("pool", lambda e: e.tensor_tensor(out=st[:, 44:48], in0=st[:, 48:52], in1=mhalf[:, 0:4], op=ALU.pow),
                      r=["yr_t", "mhalf"], w=["yr"])
                    yield
                    A("dve", lambda e: e.tensor_tensor(out=yy[:].rearrange("p (g d) -> p g d", g=4),
                                                       in0=yy[:].rearrange("p (g d) -> p g d", g=4),
                                                       in1=st[:, 44:48].unsqueeze(2).to_broadcast([128, 4, 128]), op=ALU.mult),
                      r=[("yy", h) for h in range(4)] + ["yr"], w=[("yy", h) for h in range(4)])
                    yield
                    A("pool", lambda e: e.tensor_tensor(out=gmx[:], in0=yy[:], in1=gml[:, 1024:1536], op=ALU.mult),
                      r=[("yy", h) for h in range(4)] + ["gml"], w=["gmx"])
                    yield

                def G3_gen(t):
                    for h in range(4):
                        A("pe", lambda e, h=h: e.transpose(out=PSB[7][:, h * 128:(h + 1) * 128],
                                                           in_=gmx[:, h * 128:(h + 1) * 128], identity=identb[:]),
                          r=["gmx", "identb"], w=[pk(7)])
                    yield
                    yield
                    tt = t - 2
                    A("act", lambda e: e.activation(out=gmT[:, :, tt * 128:(tt + 1) * 128],
                                                    in_=PSB[7][:, 0:512].rearrange("p (h t) -> p h t", h=4), func=AF.Copy),
                      r=[pk(7)], w=[("gmT", tt)])
                    yield

                b1_load_x(0)
                b1_load_x(1)
                b1_load_cs(2)
                round_robin([Fa1_gen(0)])
                b1_load_x(2)
                round_robin([Fa2_gen(0), Fa1_gen(1)])
                b1_load_x(3)
                round_robin([Fb_gen(0), Fa2_gen(1), Fa1_gen(2)])
                def delayed(gen, n):
                    for _ in range(n):
                        yield
                    yield from gen

                for t in range(NKC + 2):
                    tails = []
                    if 0 <= t - 1 < NKC:
                        tails.append(Kt_gen(t - 1))
                    if 2 <= t - 2 < NKC:
                        tails.append(G3_gen(t - 2))
                    if 2 <= t - 1 < NKC:
                        tails.append(G2_gen(t - 1))
                    if t >= NKC:
                        round_robin(tails)
                        continue
                    gens = [K_gen(t)]
                    if t >= 2:
                        gens.append(G_gen(t))
                    if t + 1 < NKC:
                        gens.append(Fb_gen(t + 1))
                    if t + 2 < NKC:
                        gens.append(Fa2_gen(t + 2))
                    if t + 3 < NKC:
                        gens.append(Fa1_gen(t + 3))
                    gens += [delayed(g_, 4) for g_ in tails]
                    round_robin(gens)
                    if t + 4 < NKC:
                        b1_load_x(t + 4)
                    if t + 3 < NKC:
                        b1_load_cs(t + 3)
                if debug:
                    A("pool", lambda e: e.dma_start(out=dbg["d_kt"], in_=KT[:].rearrange("p h t -> p (h t)")),
                      r=[("KT", t) for t in range(NKC)], chan="d_kt")
                    A("pool", lambda e: e.dma_start(out=dbg["d_v1"], in_=V1[:].rearrange("p a b c -> p (a b c)")),
                      r=[("V1", t) for t in range(NKC)], chan="d_v1")
                    A("pool", lambda e: e.dma_start(out=dbg["d_gmt"], in_=gmT[:].rearrange("p h t -> p (h t)")),
                      r=[("gmT", t) for t in range(NT)], chan="d_gmt")
                S.emit()

            with ExitStack() as p2:
                wq = sb("wq", [128, 8, 512], BF16, p2)
                wo = sb("wo", [128, 8, 1024], BF16, p2)
                A("pool", lambda e: e.dma_start(out=wq[:], in_=win_d[:, 0:512].rearrange("(k p) n -> p k n", p=128)),
                  w=["wq"], chan="wq")
                for i in range(2):
                    A("pool", lambda e, i=i: e.dma_start(out=wo[:, :, i * 512:(i + 1) * 512],
                                                         in_=wout_d[:, i * 512:(i + 1) * 512].rearrange("(k p) n -> p k n", p=128)),
                      w=[("wo", i)], chan=("wo", i))
                subg = sb("subg", [128, 128], F32, p2)
                load("sp", "subg", subg[:], subg_d)
                gm_b = sb("gm_b", [128, 1024], F32, p2)
                bcast_rows(gm_b, 16, "gm_b")
                xt = [sb("xq%d" % i, [128, 1024], F32, p2) for i in range(2)]
                xo = sb("xo", [128, 1024], F32, p2)
                cst = [sb("csq%d" % i, [128, 64], F32, p2) for i in range(2)]
                xn = sb("xnq", [128, 1024], BF16, p2)
                hxT = sb("hxTq", [128, 8, 128], BF16, p2)
                st = sb("stq", [128, 64], F32, p2)
                t0 = sb("t0q", [128, 512], F32, p2)
                t1 = sb("t1q", [128, 512], F32, p2)
                t2 = sb("t2q", [128, 1024], F32, p2)
                qr = sb("qr", [128, 512], BF16, p2)
                QTa = [sb("QTa%d" % i, [128, 4, 512], BF16, p2) for i in range(2)]
                pt12 = [sb("pt12_%d" % i, [128, 1024], BF16, p2) for i in range(2)]
                pt1 = [pt12[i][:, 0:512] for i in range(2)]
                pt2 = [pt12[i][:, 512:1024] for i in range(2)]
                ax = [sb("ax%d" % i, [128, 4, 512], BF16, p2) for i in range(2)]
                axT = sb("axT", [128, 4, 128], BF16, p2)
                x1t = sb("x1t", [128, 1024], F32, p2)
                ot = sb("ot", [128, 4, 128], F32, p2)
                oo = sb("oo", [128, 4, 128], F32, p2)
                pst = sb("pst", [128, 8, 8], F32, p2)

                def acc(m, s):
                    i = m * 4 + s
                    bank = 5 + i // 3
                    c0 = (i % 3) * 130
                    return bank, PS[bank][:, c0:c0 + 129]

                nload = [0]

                def prep_gen(qt):
                    qb = qt % 2
                    for s in range(4):
                        tok0 = qt * 512 + s * 128
                        bf = nload[0] % 2
                        nload[0] += 1
                        A("sp", lambda e, bf=bf, tok0=tok0: e.dma_start(out=xt[bf][:], in_=x_d[tok0:tok0 + 128, :]),
                          w=[("xq", bf)], chan=("xq", bf))
                        A("sp", lambda e, bf=bf, tok0=tok0: e.dma_start(out=cst[bf][:, 0:32], in_=cos_d[tok0:tok0 + 128, :]),
                          w=[("csq", bf, 0)], chan=("csq", bf, 0))
                        A("sp", lambda e, bf=bf, tok0=tok0: e.dma_start(out=cst[bf][:, 32:64], in_=sin_d[tok0:tok0 + 128, :]),
                          w=[("csq", bf, 1)], chan=("csq", bf, 1))
                        yield
                        xnorm(xt[bf][:], ("xq", bf), st[:, 0:1], st[:, 1:2], st[:, 2:3], xn[:], "b2", on_dve=True)
                        for _ in range(8):
                            yield
                        fm_transposes(xn, "b2xn", 4)
                        for _ in range(3):
                            yield
                        to_featmajor(xn, "b2xn", 4, lambda k, j: smix3[:, k, j:j + 1], lambda k, j: mod3[:, k, j:j + 1], 0,
                                     hxT, "hxTq", do_transposes=False, dve_ks=tuple(range(8)))
                        for _ in range(4):
                            yield
                        for k in range(8):
                            A("pe", lambda e, k=k: e.matmul(PS[4][:], lhsT=hxT[:, k, :], rhs=wq[:, k, :],
                                                            start=(k == 0), stop=(k == 7)),
                              r=["hxTq", "wq"], w=[pk(4)])
                        for _ in range(4):
                            yield
                        for _ in qknorm_rope_gen(PS[4][:], pk(4), 0, True, cst[bf][:, 0:32], cst[bf][:, 32:64],
                                                 [("csq", bf, 0), ("csq", bf, 1)],
                                                 (t0[:], t1[:], t2[:], st[:, 8:16], st[:, 16:24], st[:, 24:32]), qr[:], "qr", "q",
                                                 engs=("dve",) * 5):
                            yield
                            yield
                            yield
                        for h in range(4):
                            A("pe", lambda e, h=h: e.transpose(out=PSB[4][:, h * 128:(h + 1) * 128],
                                                               in_=qr[:, h * 128:(h + 1) * 128], identity=identb[:]),
                              r=["qr", "identb"], w=[pk(4)])
                        for _ in range(3):
                            yield
                        pq = PSB[4][:, 0:512].rearrange("p (h t) -> p h t", h=4)
                        A("dve", lambda e, s=s, pq=pq: e.tensor_copy(out=QTa[qb][:, :, s * 128:(s + 1) * 128], in_=pq),
                          r=[pk(4)], w=[("QTa", qb, s)])
                        yield

                def oproj_gen(qt):
                    qb = qt % 2
                    for s in range(4):
                        tok0 = qt * 512 + s * 128
                        tile = qt * 4 + s
                        A("sp", lambda e, tok0=tok0: e.dma_start(out=xo[:], in_=x_d[tok0:tok0 + 128, :]), w=["xo"], chan="xo")
                        for h in range(4):
                            A("pe", lambda e, h=h, s=s: e.transpose(out=PSB[4][:, h * 128:(h + 1) * 128],
                                                                    in_=ax[qb][:, s, h * 128:(h + 1) * 128], identity=identb[:]),
                              r=[("ax", qb, s, h), "identb"], w=[pk(4)])
                        for _ in range(3):
                            yield
                        A("dve", lambda e: e.tensor_copy(out=axT[:], in_=PSB[4][:, 0:512].rearrange("p (h t) -> p h t", h=4)),
                          r=[pk(4)], w=["axT"])
                        for _ in range(3):
                            yield
                        for q4 in range(4):
                            for k in range(8):
                                if k < 4:
                                    lhs = axT[:, k, :]
                                    rk = "axT"
                                else:
                                    lhs = gmT[:, k - 4, tok0:tok0 + 128]
                                    rk = ("gmT", tile)
                                A("pe", lambda e, lhs=lhs, k=k, q4=q4: e.matmul(
                                    PS[4][:, 256:512], lhsT=lhs, rhs=wo[:, k, q4 * 256:(q4 + 1) * 256],
                                    start=(k == 0), stop=(k == 7)),
                                  r=[rk, ("wo", q4 // 2)], w=[pk(4)])
                            A("dve", lambda e, q4=q4: e.tensor_tensor(
                                out=x1t[:, q4 * 256:(q4 + 1) * 256], in0=PS[4][:, 256:512],
                                in1=gm_b[:, q4 * 256:(q4 + 1) * 256], op=ALU.mult),
                              r=[pk(4), "gm_b"], w=[("x1t", q4)])
                            yield
                            yield
                        A("pool", lambda e: e.tensor_tensor(out=x1t[:], in0=x1t[:], in1=xo[:], op=ALU.add),
                          r=[("x1t", q4) for q4 in range(4)] + ["xo"], w=[("x1t", q4) for q4 in range(4)])
                        A("sp", lambda e, tok0=tok0: e.dma_start(out=x1_d[tok0:tok0 + 128, :], in_=x1t[:]),
                          r=[("x1t", q4) for q4 in range(4)], w=[("x1s", tile)], chan="x1st")
                        yield

                def attention(qt, side):
                    qb = qt % 2
                    qa_keys = [("QTa", qb, s) for s in range(4)]
                    qb_keys = [("QTb", qb, s) for s in range(4)]
                    side = list(side)

                    def tick():
                        while side:
                            try:
                                next(side[0])
                                return
                            except StopIteration:
                                side.pop(0)

                    for h in range(4):
                        def qk(kc, h=h):
                            b = kc % 2
                            A("pe", lambda e: e.matmul(PS[2 * b][:], lhsT=KT[0:64, h, kc * 128:(kc + 1) * 128], rhs=QTa[qb][0:64, h, :],
                                                       start=True, stop=True), r=[("KT", kc)] + qa_keys, w=[pk(2 * b)])
                            A("pe", lambda e: e.matmul(PS[2 * b + 1][:], lhsT=KT[64:128, h, kc * 128:(kc + 1) * 128], rhs=QTa[qb][64:128, h, :],
                                                       start=True, stop=True), r=[("KT", kc)] + qa_keys, w=[pk(2 * b + 1)])
                            A("act", lambda e: e.activation(out=pt12[b][:], in_=PS2[b][:], func=AF.Exp, scale=0.125),
                              r=[pk(2 * b), pk(2 * b + 1)], w=[("pt1", b), ("pt2", b)])

                        def pv(kc, h=h):
                            b = kc % 2
                            for m in range(2):
                                pt = pt1[b] if m == 0 else pt2[b]
                                for s in range(4):
                                    bank, ap = acc(m, s)
                                    first = (kc == 0) and ((m * 4 + s) % 3 == 0)
                                    A("pe", lambda e, pt=pt, s=s, ap=ap, first=first: e.matmul(
                                        ap, lhsT=pt[:, s * 128:(s + 1) * 128], rhs=V1[:, kc, h, 0:129],
                                        start=first, stop=(kc == NKC - 1), skip_group_check=True),
                                      r=[("pt1" if m == 0 else "pt2", b), ("V1", kc)], w=[pk(bank)])

                        qk(0)
                        for kc in range(NKC):
                            if kc + 1 < NKC:
                                qk(kc + 1)
                            pv(kc)
                            tick()
                            tick()
                        for s in range(4):
                            for m in range(2):
                                bk, a = acc(m, s)
                                A("dve", lambda e, s=s, m=m, a=a: e.reciprocal(out=pst[:, s, m:m + 1], in_=a[:, 128:129]),
                                  r=[pk(bk)], w=[("rr", s, m)])
                        for s in range(4):
                            A("dve", lambda e, s=s: e.tensor_tensor(out=pst[:, s, 2:3], in0=pst[:, s, 1:2], in1=nlam, op=ALU.mult),
                              r=[("rr", s, 1), "nlam"], w=[("rr", s, 2)])
                        for s in range(4):
                            bk, a1 = acc(0, s)
                            A("dve", lambda e, s=s, a1=a1: e.tensor_scalar(out=ot[:, s, :], in0=a1[:, 0:128], scalar1=pst[:, s, 0:1],
                                                                         scalar2=None, op0=ALU.mult),
                              r=[pk(bk), ("rr", s, 0)], w=[("ot", s)])
                        for s in range(4):
                            bk, a2 = acc(1, s)
                            A("dve", lambda e, s=s, a2=a2: e.scalar_tensor_tensor(out=oo[:, s, :], in0=a2[:, 0:128], scalar=pst[:, s, 2:3],
                                                                                 in1=ot[:, s, :], op0=ALU.mult, op1=ALU.add),
                              r=[pk(bk), ("rr", s, 2), ("ot", s)], w=[("oo", s)])
                        for s in range(4):
                            A("dve", lambda e, s=s: e.scalar_tensor_tensor(out=junk2[:, s * 128:(s + 1) * 128], in0=oo[:, s, :], scalar=1.0,
                                                                           in1=oo[:, s, :], op0=ALU.mult, op1=ALU.mult,
                                                                           accum_out=pst[:, s, 3:4]),
                              r=[("oo", s)], w=[("junk2", s), ("rr", s, 3)])
                        c = (1.0 - LAMBDA_INIT) ** 2
                        for s in range(4):
                            rsq(pst[:, s, 4:5], pst[:, s, 3:4], 1, 1.0 / (128 * c), EPS / c, pst[:, s, 5:6], [("rr", s, 3)], "rr4_%d" % s)
                        for s in range(4):
                            A("dve", lambda e, s=s, h=h: e.scalar_tensor_tensor(
                                out=ax[qb][:, s, h * 128:(h + 1) * 128], in0=oo[:, s, :], scalar=pst[:, s, 4:5], in1=subg[:],
                                op0=ALU.mult, op1=ALU.mult),
                              r=[("oo", s), "rr4_%d" % s, "subg"], w=[("ax", qb, s, h)])
                    while side:
                        tick()

                for _ in prep_gen(0):
                    pass
                for qt in range(8):
                    side = []
                    if qt >= 1:
                        side.append(oproj_gen(qt - 1))
                    if qt + 1 < 8:
                        side.append(prep_gen(qt + 1))
                    attention(qt, side)
                for _ in oproj_gen(7):
                    pass
                S.emit()

        with ExitStack() as pc:
            fxT = sb("fxT", [128, 8, 2048], BF16, pc)
            accm = sb("accm", [128, 16, 1024], F32, pc)
            gates = sb("gates", [128, 16, 32], F32, pc)
            xt = [sb("xc%d" % i, [128, 1024], F32, pc) for i in range(2)]
            xn = sb("xnc", [128, 1024], BF16, pc)
            st = sb("stc", [128, 16], F32, pc)
            wgu = [sb("wgu%d" % i, [128, 8, 512], BF16, pc) for i in range(2)]
            wd = [sb("wd%d" % i, [128, 2, 1024], BF16, pc) for i in range(2)]
            sg = [sb("sg%d" % i, [128, 512], BF16, pc) for i in range(2)]
            hmT = [sb("hmT%d" % i, [128, 2, 512], BF16, pc) for i in range(2)]
            rt = sb("rt", [128, 8, 32], F32, pc)
            m8 = sb("m8", [128, 6, 8], F32, pc)
            grp8 = sb("grp8", [128, 8], F32, pc)
            ofin = [sb("ofin%d" % i, [128, 1024], F32, pc) for i in range(2)]
            gf_b = sb("gf_b", [128, 1024], F32, pc)
            bcast_rows(gf_b, 40, "gf_b")
            A("dve", lambda e: e.memset(grp8[:], -1e30), w=["grp8"])
            out_ops = []
            NP = 4
            xt = xt + [sb("xc%d" % i, [128, 1024], F32, pc) for i in range(2, NP)]
            xn2 = [xn] + [sb("xnc%d" % i, [128, 1024], BF16, pc) for i in range(1, NP)]
            st2 = [st] + [sb("stc%d" % i, [128, 16], F32, pc) for i in range(1, NP)]
            rt2 = [rt] + [sb("rt%d" % i, [128, 8, 32], F32, pc) for i in range(1, NP)]
            m82 = [m8] + [sb("m8_%d" % i, [128, 6, 8], F32, pc) for i in range(1, NP)]
            grp82 = [grp8] + [sb("grp8_%d" % i, [128, 8], F32, pc) for i in range(1, NP)]
            xf = [sb("xf%d" % i, [128, 1024], F32, pc) for i in range(2)]
            for i in range(NP):
                A("dve", lambda e, i=i: e.memset(grp82[i][:], -1e30), w=[("grp8", i)])

            def P_gen(hf, t):
                q = t % NP
                tile = hf * 16 + t
                tok0 = tile * 128
                xt_, xn_, st_, rt_, m8_, g8_ = xt[q], xn2[q], st2[q], rt2[q], m82[q], grp82[q]
                b0, b1 = 2 * q, 2 * q + 1
                K = lambda nm: (nm, q)
                A("sp", lambda e: e.dma_start(out=xt_[:], in_=x1_d[tok0:tok0 + 128, :]),
                  r=[("x1s", tile)], w=[("xc", q)], chan=("xc", q))
                yield
                A("act", lambda e: e.activation(out=junk[:], in_=xt_[:], func=AF.Square, accum_out=st_[:, 0:1]),
                  r=[("xc", q)], w=["junk", K("ssq")])
                yield
                A("dve", lambda e: e.tensor_scalar(out=st_[:, 2:3], in0=st_[:, 0:1], scalar1=1.0 / D, scalar2=EPS, op0=ALU.mult, op1=ALU.add),
                  r=[K("ssq")], w=[K("rstd_t")])
                yield
                A("pool", lambda e: e.tensor_tensor(out=st_[:, 1:2], in0=st_[:, 2:3], in1=mhalf[:, 0:1], op=ALU.pow),
                  r=[K("rstd_t"), "mhalf"], w=[K("rstd")])
                yield
                A("dve", lambda e: e.tensor_scalar(out=xn_[:], in0=xt_[:], scalar1=st_[:, 1:2], scalar2=None, op0=ALU.mult),
                  r=[("xc", q), K("rstd")], w=[K("xn")])
                yield
                for k in range(8):
                    A("pe", lambda e, k=k: e.transpose(out=PSB[b0][:, k * 128:(k + 1) * 128],
                                                       in_=xn_[:, k * 128:(k + 1) * 128], identity=identb[:]),
                      r=[K("xn"), "identb"], w=[pk(b0)])
                yield
                for k in range(8):
                    if k % 2 == 1:
                        A("dve", lambda e, k=k: e.tensor_scalar(out=fxT[:, k, t * 128:(t + 1) * 128],
                                                                in0=PSB[b0][:, k * 128:(k + 1) * 128],
                                                                scalar1=sffn[:, k:k + 1], scalar2=mod3[:, 24 + k, 0:1],
                                                                op0=ALU.mult, op1=ALU.add),
                          r=[pk(b0), "sffn", "mod"], w=[("fxT", t)])
                    else:
                        A("act", lambda e, k=k: e.activation(out=fxT[:, k, t * 128:(t + 1) * 128],
                                                             in_=PSB[b0][:, k * 128:(k + 1) * 128], func=AF.Identity,
                                                             scale=sffn[:, k:k + 1], bias=mod3[:, 24 + k, 0:1]),
                          r=[pk(b0), "sffn", "mod"], w=[("fxT", t)])
                yield
                for k in range(8):
                    A("pe", lambda e, k=k: e.matmul(PS[b1][:, 0:32], lhsT=fxT[:, k, t * 128:(t + 1) * 128], rhs=wr[:, k, :],
                                                    start=(k == 0), stop=(k == 7)),
                      r=[("fxT", t), "wr"], w=[pk(b1)])
                yield
                sc = rt_[:, 0, :]
                bi = rt_[:, 1, :]
                b10 = rt_[:, 2, :]
                msk = rt_[:, 3, :]
                sel = rt_[:, 4, :]
                ww = rt_[:, 5, :]
                gmask = rt_[:, 6, 0:4]
                A("act", lambda e: e.activation(out=sc, in_=PS[b1][:, 0:32], func=AF.Sigmoid), r=[pk(b1)], w=[K("sc")])
                yield
                A("dve", lambda e: e.tensor_tensor(out=bi, in0=sc, in1=rbias[:], op=ALU.add), r=[K("sc"), "rbias"], w=[K("bi")])
                yield
                for gi in range(4):
                    A("dve", lambda e, gi=gi: e.max(out=m8_[:, gi, :], in_=bi[:, gi * 8:(gi + 1) * 8]), r=[K("bi")], w=[("m8", q, gi)])
                A("dve", lambda e: e.tensor_scalar(out=b10, in0=bi, scalar1=10.0, scalar2=None, op0=ALU.add), r=[K("bi")], w=[K("b10")])
                yield
                A("dve", lambda e: e.tensor_tensor(out=g8_[:, 0:4], in0=m8_[:, 0:4, 0], in1=m8_[:, 0:4, 1], op=ALU.add),
                  r=[("m8", q, gi) for gi in range(4)] + [("grp8", q)], w=[K("grp")])
                yield
                A("dve", lambda e: e.max(out=m8_[:, 4, :], in_=g8_[:]), r=[K("grp")], w=[("m8", q, 4)])
                yield
                A("dve", lambda e: e.tensor_scalar(out=gmask, in0=g8_[:, 0:4], scalar1=m8_[:, 4, 1:2], scalar2=None, op0=ALU.is_ge),
                  r=[K("grp"), ("m8", q, 4)], w=[K("gmask")])
                yield
                A("dve", lambda e: e.tensor_tensor(
                    out=msk.rearrange("p (g i) -> p g i", g=4), in0=b10.rearrange("p (g i) -> p g i", g=4),
                    in1=gmask.unsqueeze(2).to_broadcast([128, 4, 8]), op=ALU.mult), r=[K("b10"), K("gmask")], w=[K("msk")])
                yield
                A("dve", lambda e: e.max(out=m8_[:, 5, :], in_=msk), r=[K("msk")], w=[("m8", q, 5)])
                yield
                A("dve", lambda e: e.tensor_scalar(out=sel, in0=msk, scalar1=m8_[:, 5, 3:4], scalar2=None, op0=ALU.is_ge),
                  r=[K("msk"), ("m8", q, 5)], w=[K("sel")])
                yield
                A("dve", lambda e: e.scalar_tensor_tensor(out=ww, in0=sc, scalar=1.0, in1=sel, op0=ALU.mult, op1=ALU.mult,
                                                          accum_out=st_[:, 4:5]),
                  r=[K("sc"), K("sel")], w=[K("ww"), K("wsum")])
                yield
                A("dve", lambda e: e.reciprocal(out=st_[:, 5:6], in_=st_[:, 4:5]), r=[K("wsum")], w=[K("rws")])
                yield
                A("dve", lambda e: e.tensor_scalar(out=gates[:, t, :], in0=ww, scalar1=st_[:, 5:6], scalar2=2.5,
                                                   op0=ALU.mult, op1=ALU.mult),
                  r=[K("ww"), K("rws")], w=[("gates", t)])
                yield

            def Fin_gen(hf, t):
                q = t % 2
                tile = hf * 16 + t
                tok0 = tile * 128
                A("sp", lambda e: e.dma_start(out=xf[q][:], in_=x1_d[tok0:tok0 + 128, :]),
                  r=[("x1s", tile)], w=[("xf", q)], chan=("xf", q))
                yield
                A("dve", lambda e: e.tensor_tensor(out=ofin[q][:], in0=accm[:, t, :], in1=gf_b[:], op=ALU.mult),
                  r=[("accm", t, 0), ("accm", t, 1), "gf_b"], w=[("ofin", q)])
                yield
                A("pool", lambda e: e.tensor_tensor(out=ofin[q][:], in0=ofin[q][:], in1=xf[q][:], op=ALU.add),
                  r=[("ofin", q), ("xf", q)], w=[("ofin", q)])
                yield
                out_ops.append(A("sp", lambda e: e.dma_start(out=out_d[tok0:tok0 + 128, :], in_=ofin[q][:]),
                                 r=[("ofin", q)], chan=("ost", q)))
                yield

            def seq(*gens):
                for g_ in gens:
                    yield from g_

            for i in range(4):
                round_robin([P_gen(0, 4 * i + j) for j in range(4)])
            for hf in range(2):
                fx_keys = [("fxT", t) for t in range(16)]

                def wload(e_i):
                    bf = e_i % 2
                    if e_i < NEXP:
                        sg_, su_, sd_ = weg_d[e_i], weu_d[e_i], wed_d[e_i]
                    else:
                        sg_, su_, sd_ = wsg_d, wsu_d, wsd_d
                    A("pool", lambda e: e.dma_start(out=wgu[bf][:, :, 0:256], in_=sg_.rearrange("(k p) f -> p k f", p=128)),
                      w=[("wg", bf)], chan=("wg", bf))
                    A("pool", lambda e: e.dma_start(out=wgu[bf][:, :, 256:512], in_=su_.rearrange("(k p) f -> p k f", p=128)),
                      w=[("wu", bf)], chan=("wu", bf))
                    A("pool", lambda e: e.dma_start(out=wd[bf][:], in_=sd_.rearrange("(k p) n -> p k n", p=128)),
                      w=[("wd", bf)], chan=("wd", bf))

                if hf == 0:
                    wload(0)
                    wload(1)
                dcount = [0]

                def gu_group(e_i, tc, g):
                    bf = e_i % 2
                    hb = (e_i * 4 + tc) % 2
                    mat, fc = divmod(g, 2)
                    bank = g
                    for k in range(8):
                        A("pe", lambda e, mat=mat, fc=fc, k=k, bank=bank: e.matmul(
                            PS[bank][:], lhsT=wgu[bf][:, k, mat * 256 + fc * 128:mat * 256 + (fc + 1) * 128],
                            rhs=fxT[:, k, tc * 512:(tc + 1) * 512], start=(k == 0), stop=(k == 7)),
                          r=[("wg" if mat == 0 else "wu", bf)] + fx_keys[tc * 4:(tc + 1) * 4], w=[pk(bank)])
                    if mat == 0:
                        A("act", lambda e, fc=fc: e.activation(out=sg[fc][:], in_=PS[fc][:], func=AF.Silu), r=[pk(fc)], w=[("sg", fc)])
                    else:
                        A("dve", lambda e, fc=fc: e.tensor_tensor(out=hmT[hb][:, fc, :], in0=sg[fc][:], in1=PS[2 + fc][:], op=ALU.mult),
                          r=[("sg", fc), pk(2 + fc)], w=[("hmT", hb, fc)])

                def gu(e_i, tc):
                    for g in range(4):
                        gu_group(e_i, tc, g)

                def down(e_i, tc, nxt=None):
                    bf = e_i % 2
                    hb = (e_i * 4 + tc) % 2
                    for s in range(4):
                        if nxt is not None:
                            gu_group(nxt[0], nxt[1], s)
                        t = tc * 4 + s
                        db = dcount[0] % 2
                        dcount[0] += 1
                        for half in range(2):
                            bank = 4 + db * 2 + half
                            for fc in range(2):
                                A("pe", lambda e, fc=fc, half=half, bank=bank, s=s: e.matmul(
                                    PS[bank][:], lhsT=hmT[hb][:, fc, s * 128:(s + 1) * 128],
                                    rhs=wd[bf][:, fc, half * 512:(half + 1) * 512], start=(fc == 0), stop=(fc == 1)),
                                  r=[("hmT", hb, fc), ("wd", bf)], w=[pk(bank)])
                            dst = accm[:, t, half * 512:(half + 1) * 512]
                            if e_i == 0:
                                A("dve", lambda e, dst=dst, bank=bank, t=t: e.tensor_scalar(
                                    out=dst, in0=PS[bank][:], scalar1=gates[:, t, e_i:e_i + 1], scalar2=None, op0=ALU.mult),
                                  r=[pk(bank), ("gates", t)], w=[("accm", t, half)])
                            elif e_i < NEXP:
                                A("dve", lambda e, dst=dst, bank=bank, t=t: e.scalar_tensor_tensor(
                                    out=dst, in0=PS[bank][:], scalar=gates[:, t, e_i:e_i + 1], in1=dst, op0=ALU.mult, op1=ALU.add),
                                  r=[pk(bank), ("gates", t), ("accm", t, half)], w=[("accm", t, half)])
                            else:
                                A("dve", lambda e, dst=dst, bank=bank: e.tensor_tensor(out=dst, in0=PS[bank][:], in1=dst, op=ALU.add),
                                  r=[pk(bank), ("accm", t, half)], w=[("accm", t, half)])

                units = [(e_i, tc) for e_i in range(NEXP + 1) for tc in range(4)]
                gu(*units[0])
                for i, u in enumerate(units):
                    down(u[0], u[1], units[i + 1] if i + 1 < len(units) else None)
                    if u[1] == 3 and u[0] + 2 <= NEXP:
                        wload(u[0] + 2)
                    if hf == 1 and u[0] == NEXP:
                        tc_ = u[1]
                        round_robin([Fin_gen(1, tc_ * 4), Fin_gen(1, tc_ * 4 + 1)])
                        round_robin([Fin_gen(1, tc_ * 4 + 2), Fin_gen(1, tc_ * 4 + 3)])
                if hf == 0:
                    wload(0)
                    wload(1)
                    for i in range(4):
                        round_robin([seq(Fin_gen(0, 4 * i), Fin_gen(0, 4 * i + 2)), seq(Fin_gen(0, 4 * i + 1), Fin_gen(0, 4 * i + 3))]
                                    + [P_gen(1, 4 * i + j) for j in range(4)])
                else:
                    pass
            extra = list(out_ops) + list(S.chan_last.values())
            A("sp", lambda e: e.nop(), extra=extra)
            S.emit()
    return nc


def rope_tables():
    rows = L // 64
    row = np.repeat(np.arange(rows, dtype=np.float32), 64)
    col = np.tile(np.arange(64, dtype=np.float32), rows)
    half = 32
    inv_freq = (np.float32(10000.0) ** (-np.arange(0, half, 2, dtype=np.float32) / np.float32(half))).astype(np.float32)
    ang = np.concatenate([row[:, None] * inv_freq, col[:, None] * inv_freq], axis=-1).astype(np.float32)
    return np.cos(ang).astype(np.float32), np.sin(ang).astype(np.float32)


def make_in_maps(inp, cores):
    f = lambda a: np.ascontiguousarray(np.asarray(a, dtype=np.float32))
    cos, sin = rope_tables()
    bc = lambda v: f(np.broadcast_to(np.asarray(v, np.float32).reshape(1, -1), (128, np.asarray(v).size)))
    col = lambda v, n: f(np.asarray(v, np.float32).reshape(n, 128).T)
    shared = {
        "w_ada": f(inp["w_ada"][0]),
        "b_adaT": col(inp["b_ada"][0], 48),
        "nmg": col(inp["norm_mix_g"][0], 8),
        "nfg": col(inp["norm_ffn_g"][0], 8),
        "w_in": f(inp["w_in"][0]),
        "qkg": bc(np.concatenate([np.asarray(inp["q_norm_g"][0]), np.asarray(inp["k_norm_g"][0])])),
        "lam_in": bc(np.asarray(inp["da_lambda"][0]).reshape(-1)),
        "subg": bc(inp["subln_g"][0]),
        "gml": bc(np.concatenate([np.asarray(inp["gm_ln_g"][0]).reshape(-1), np.asarray(inp["gm_ln_b"][0]).reshape(-1),
                                  np.asarray(inp["gm_out_g"][0]).reshape(-1)])),
        "w_sT": f(np.transpose(np.asarray(inp["gm_ws"][0]), (2, 0, 1)).reshape(128, 512)),
        "bsT": f(np.asarray(inp["gm_bs"][0]).T),
        "w_out": f(inp["w_out"][0]),
        "w_router": f(inp["w_router"][0]),
        "rbias": bc(inp["router_bias"][0]),
        "we_gate": f(inp["we_gate"][0]),
        "we_up": f(inp["we_up"][0]),
        "we_down": f(inp["we_down"][0]),
        "ws_gate": f(inp["ws_gate"][0]),
        "ws_up": f(inp["ws_up"][0]),
        "ws_down": f(inp["ws_down"][0]),
        "cos": cos,
        "sin": sin,
        "ident": np.eye(128, dtype=np.float32),
    }
    maps = []
    cvec = np.asarray(inp["c"], np.float32)
    cctx = np.asarray(inp["c_ctx"], np.float32)
    for b in cores:
        m = dict(shared)
        m["x"] = f(inp["x"][b])
        m["ctx"] = f(inp["ctx"][b])
        two = np.stack([cvec[b], cctx], axis=-1).reshape(8, 128, 2)
        m["cc"] = f(np.transpose(two, (1, 0, 2)).reshape(128, 16))
        maps.append(m)
    return maps


_NC_CACHE = {}


def kernel(**inputs):
    if "nc" not in _NC_CACHE:
        _NC_CACHE["nc"] = build_program()
    nc = _NC_CACHE["nc"]
    cores = list(range(8))
    in_maps = make_in_maps(inputs, cores)
    res = run_bass_kernel_spmd(nc, in_maps, core_ids=cores)
    out = np.stack([np.asarray(res.results[b]["out"], dtype=np.float32) for b in cores], axis=0)
    return out
```

```python
import math
from contextlib import ExitStack

import numpy as np
import concourse.bass as bass
import concourse.mybir as mybir
from concourse.bass_utils import run_bass_kernel_spmd

F32 = mybir.dt.float32
BF16 = mybir.dt.bfloat16
AF = mybir.ActivationFunctionType
ALU = mybir.AluOpType
AX = mybir.AxisListType

D = 1024
L = 4096
CTX = 256
NT = L // 128
NKC = (CTX + L) // 128
EPS = 1e-6
LAMBDA_INIT = 0.8 - 0.6 * math.exp(-0.3 * 0)
NEXP = 32

ENGS = ["pe", "act", "dve", "pool", "sp"]


class Op:
    __slots__ = ("eng", "fn", "deps", "need_inc", "sem", "val", "is_dma", "chan", "emitted")


class Sched:
    def __init__(self, nc, stack):
        self.nc = nc
        self.stack = stack
        self.pending = []
        self.last_w = {}
        self.readers = {}
        self.chan_last = {}
        self.chan_count = {}
        self.cnt = {e: 0 for e in ENGS}
        self.esem = {e: stack.enter_context(nc.semaphore("s_" + e)) for e in ENGS}
        self.csem = {}
        self.known = {e: {} for e in ENGS}
        self.last_op = {}

    def add(self, eng, fn, r=(), w=(), chan=None, extra=()):
        op = Op()
        op.eng = eng
        op.fn = fn
        op.is_dma = chan is not None
        op.chan = chan
        op.need_inc = op.is_dma
        op.sem = None
        op.val = None
        op.emitted = False
        deps = set(extra)
        for b in r:
            o = self.last_w.get(b)
            if o is not None:
                deps.add(o)
        for b in w:
            o = self.last_w.get(b)
            if o is not None:
                deps.add(o)
            for o in self.readers.get(b, ()):
                deps.add(o)
        if op.is_dma and chan in self.chan_last:
            deps.add(self.chan_last[chan])
        deps.discard(op)
        for b in r:
            self.readers.setdefault(b, []).append(op)
        for b in w:
            self.last_w[b] = op
            self.readers[b] = []
        if op.is_dma:
            self.chan_last[chan] = op
            self.chan_count[chan] = self.chan_count.get(chan, 0) + 1
            op.val = 16 * self.chan_count[chan]
            if chan not in self.csem:
                self.csem[chan] = self.stack.enter_context(self.nc.semaphore("c_%d" % len(self.csem)))
            op.sem = self.csem[chan]
        else:
            op.sem = self.esem[eng]
        op.deps = deps
        self.pending.append(op)
        self.last_op[eng] = op
        return op

    @staticmethod
    def _skip(d, op):
        if d.is_dma:
            return False
        if d.emitted:
            return True
        return (not op.is_dma) and d.eng == op.eng and d.eng == "pe"

    def emit(self):
        self.add("sp", lambda e: e.nop(), extra=list(self.chan_last.values()))
        ops = self.pending
        self.pending = []
        for op in ops:
            for d in op.deps:
                if d.is_dma or self._skip(d, op):
                    continue
                d.need_inc = True
        for op in ops:
            if not op.is_dma and op.need_inc:
                self.cnt[op.eng] += 1
                op.val = self.cnt[op.eng]
        per = {e: [o for o in ops if o.eng == e] for e in ENGS}
        sched = self

        def run(name, eng):
            known = sched.known[name]
            for op in per[name]:
                waits = {}
                for d in op.deps:
                    if sched._skip(d, op):
                        continue
                    key = id(d.sem)
                    if known.get(key, 0) >= d.val:
                        continue
                    if key not in waits or waits[key][1] < d.val:
                        waits[key] = (d.sem, d.val)
                for key, (sem, val) in waits.items():
                    eng.wait_ge(sem, val)
                    known[key] = val
                inst = op.fn(eng)
                if op.need_inc:
                    inst.then_inc(op.sem, 16 if op.is_dma else 1)

        with self.nc.Block(no_gpsimd_drain=True) as block:
            @block.tensor
            def _(e):
                run("pe", e)

            @block.scalar
            def _(e):
                run("act", e)

            @block.vector
            def _(e):
                run("dve", e)

            @block.gpsimd
            def _(e):
                run("pool", e)

            @block.sync
            def _(e):
                run("sp", e)
        for op in ops:
            op.emitted = True

    def keep(self, op):
        op.need_inc = True
        return op


def build_program(debug=False):
    nc = bass.Bass("TRN2", target_bir_lowering=False)

    def din(name, shape):
        return nc.dram_tensor(name, list(shape), F32, kind="ExternalInput").ap()

    x_d = din("x", [L, D])
    ctx_d = din("ctx", [CTX, D])
    cc_d = din("cc", [128, 16])
    wada_d = din("w_ada", [D, 6 * D])
    bada_d = din("b_adaT", [128, 48])
    nmg_d = din("nmg", [128, 8])
    nfg_d = din("nfg", [128, 8])
    win_d = din("w_in", [D, 2560])
    qkg_d = din("qkg", [128, 128])
    lam_d = din("lam_in", [128, 256])
    subg_d = din("subg", [128, 128])
    gml_d = din("gml", [128, 3 * 512])
    wsT_d = din("w_sT", [128, 512])
    bsT_d = din("bsT", [128, 4])
    wout_d = din("w_out", [D, D])
    wr_d = din("w_router", [D, 32])
    rb_d = din("rbias", [128, 32])
    weg_d = din("we_gate", [NEXP, D, 256])
    weu_d = din("we_up", [NEXP, D, 256])
    wed_d = din("we_down", [NEXP, 256, D])
    wsg_d = din("ws_gate", [D, 256])
    wsu_d = din("ws_up", [D, 256])
    wsd_d = din("ws_down", [256, D])
    cos_d = din("cos", [L, 32])
    sin_d = din("sin", [L, 32])
    ident_d = din("ident", [128, 128])
    out_d = nc.dram_tensor("out", [L, D], F32, kind="ExternalOutput").ap()
    x1_d = nc.dram_tensor("x1s", [L, D], F32, kind="Internal").ap()
    dbg = {}
    if debug:
        for nm, shp in [("d_mod", [128, 96]), ("d_kt", [128, 4 * 4352]), ("d_v1", [128, 34 * 4 * 130]),
                        ("d_gmt", [128, 4 * 4096]), ("d_gates", [128, 32 * 32])]:
            dbg[nm] = nc.dram_tensor(nm, shp, F32, kind="ExternalOutput").ap()

    with ExitStack() as g:
        S = Sched(nc, g)
        A = S.add

        def sb(name, shape, dt=F32, st=None):
            return (st or g).enter_context(nc.sbuf_tensor("sb_" + name, list(shape), dt))

        PS = [g.enter_context(nc.psum_tensor("ps%d" % i, [128, 512], F32)) for i in range(8)]
        PSB = [p.bitcast(BF16) for p in PS]

        def pk(i):
            return ("ps", i)

        identf = sb("identf", [128, 128])
        identb = sb("identb", [128, 128], BF16)
        onesf = sb("onesf", [128, 128])
        mhalf = sb("mhalf", [128, 16])
        cc = sb("cc", [128, 16])
        scs = sb("scs", [128, 16])
        badaT = sb("badaT", [128, 48])
        mod = sb("mod", [128, 96])
        mod3 = mod[:].rearrange("p (c j) -> p c j", j=2)
        nmg = sb("nmg", [128, 8])
        nfg = sb("nfg", [128, 8])
        smix = sb("smix", [128, 16])
        smix3 = smix[:].rearrange("p (c j) -> p c j", j=2)
        sffn = sb("sffn", [128, 8])
        qkg = sb("qkg", [128, 128])
        lsm = sb("lsm", [128, 8])
        bsT = sb("bsT", [128, 4])
        rbias = sb("rbias", [128, 32])
        wr = sb("wr", [128, 8, 32], BF16)
        dg = sb("dg", [128, 128])
        junk = sb("junk", [128, 1024], BF16)
        junk2 = sb("junk2", [128, 512], BF16)
        junk3 = sb("junk3", [128, 1024], BF16)

        def load(eng, name, dst, src, key=None):
            return A(eng, lambda e: e.dma_start(out=dst, in_=src), w=[key or name], chan=name)

        load("sp", "identf", identf[:], ident_d)
        load("sp", "cc", cc[:], cc_d)
        load("sp", "badaT", badaT[:], bada_d)
        load("sp", "nmg", nmg[:], nmg_d)
        load("sp", "nfg", nfg[:], nfg_d)
        load("sp", "qkg", qkg[:], qkg_d)
        load("sp", "bsT", bsT[:], bsT_d)
        load("sp", "rbias", rbias[:], rb_d)
        load("pool", "wr", wr[:], wr_d.rearrange("(k p) n -> p k n", p=128))
        A("dve", lambda e: e.tensor_copy(out=identb[:], in_=identf[:]), r=["identf"], w=["identb"])
        A("dve", lambda e: e.memset(onesf[:], 1.0), w=["onesf"])
        A("dve", lambda e: e.memset(mhalf[:], -0.5), w=["mhalf"])

        def rsq(out, in_, n, mul, add, tmp, rkeys, wkey):
            A("dve", lambda e: e.tensor_scalar(out=tmp, in0=in_, scalar1=mul, scalar2=add, op0=ALU.mult, op1=ALU.add),
              r=rkeys, w=[wkey + "_t"])
            A("pool", lambda e: e.tensor_tensor(out=out, in0=tmp, in1=mhalf[:, 0:n], op=ALU.pow),
              r=[wkey + "_t", "mhalf"], w=[wkey])

        def round_robin(gens):
            gens = list(gens)
            while gens:
                for g_ in list(gens):
                    try:
                        next(g_)
                    except StopIteration:
                        gens.remove(g_)

        def bcast_rows(dst, base, key):
            for half in range(2):
                for kk in range(4):
                    k = half * 4 + kk
                    A("dve", lambda e, k=k: e.tensor_scalar(out=dg[:], in0=identf[:], scalar1=mod3[:, base + k, 0:1],
                                                            scalar2=None, op0=ALU.mult),
                      r=["identf", "mod"], w=["dg"])
                    A("pe", lambda e, kk=kk: e.matmul(PS[1][:, kk * 128:(kk + 1) * 128], lhsT=onesf[:], rhs=dg[:],
                                                      start=True, stop=True), r=["onesf", "dg"], w=[pk(1)])
                A("dve", lambda e, half=half: e.tensor_copy(out=dst[:, half * 512:(half + 1) * 512], in_=PS[1][:]),
                  r=[pk(1)], w=[key])

        with ExitStack() as pa:
            lam_in = sb("lam_in", [128, 256], F32, pa)
            load("sp", "lam_in", lam_in[:], lam_d)
            wa = [sb("wa%d" % i, [128, 8, 512], F32, pa) for i in range(2)]
            A("act", lambda e: e.activation(out=scs[:], in_=cc[:], func=AF.Silu), r=["cc"], w=["scs"])
            for c in range(12):
                bf = c % 2
                A("sp", lambda e, c=c, bf=bf: e.dma_start(
                    out=wa[bf][:], in_=wada_d[:, c * 512:(c + 1) * 512].rearrange("(k p) n -> p k n", p=128)),
                  w=[("wa", bf)], chan=("wa", bf))
                for j in range(4):
                    col = c * 4 + j
                    for k in range(8):
                        A("pe", lambda e, bf=bf, j=j, k=k, col=col: e.matmul(
                            PS[0][:, col * 2:col * 2 + 2], lhsT=wa[bf][:, k, j * 128:(j + 1) * 128],
                            rhs=scs[:, 2 * k:2 * k + 2], start=(k == 0), stop=(k == 7)),
                          r=[("wa", bf), "scs"], w=[pk(0)])
            ps0v = PS[0][:, 0:96].rearrange("p (c j) -> p c j", j=2)
            for j in range(2):
                A("dve", lambda e, j=j: e.tensor_tensor(out=mod3[:, :, j], in0=ps0v[:, :, j], in1=badaT[:], op=ALU.add),
                  r=[pk(0), "badaT"], w=["mod"])
            for j in range(2):
                A("dve", lambda e, j=j: e.scalar_tensor_tensor(out=smix3[:, :, j], in0=mod3[:, 8:16, j], scalar=1.0,
                                                                in1=nmg[:], op0=ALU.add, op1=ALU.mult),
                  r=["mod", "nmg"], w=["smix"])
            A("dve", lambda e: e.scalar_tensor_tensor(out=sffn[:], in0=mod3[:, 32:40, 0], scalar=1.0, in1=nfg[:],
                                                      op0=ALU.add, op1=ALU.mult), r=["mod", "nfg"], w=["sffn"])
            for i in range(2):
                A("dve", lambda e, i=i: e.scalar_tensor_tensor(
                    out=junk2[:, 0:64], in0=lam_in[:, i * 128:i * 128 + 64], scalar=1.0,
                    in1=lam_in[:, i * 128 + 64:i * 128 + 128], op0=ALU.mult, op1=ALU.mult, accum_out=lsm[:, i:i + 1]),
                  r=["lam_in"], w=["junk2", ("lsm", i)])
            A("act", lambda e: e.activation(out=lsm[:, 2:4], in_=lsm[:, 0:2], func=AF.Exp), r=[("lsm", 0), ("lsm", 1)], w=["lse"])
            A("dve", lambda e: e.tensor_tensor(out=lsm[:, 4:5], in0=lsm[:, 3:4], in1=lsm[:, 2:3], op=ALU.subtract),
              r=["lse"], w=["nl0"])
            A("dve", lambda e: e.tensor_scalar(out=lsm[:, 5:6], in0=lsm[:, 4:5], scalar1=-LAMBDA_INIT, scalar2=None, op0=ALU.add),
              r=["nl0"], w=["nlam"])
            nlam = lsm[:, 5:6]
            if debug:
                dm = A("sp", lambda e: e.dma_start(out=dbg["d_mod"], in_=mod[:]), r=["mod"], chan="d_mod")
            S.emit()

        with ExitStack() as pb:
            KT = sb("KT", [128, 4, NKC * 128], BF16, pb)
            V1 = sb("V1", [128, NKC, 4, 130], BF16, pb)
            gmT = sb("gmT", [128, 4, L], BF16, pb)
            A("pool", lambda e: e.memset(V1[:].rearrange("p a b c -> p (a b c)"), 1.0), w=["V1init"])

            def xnorm(xt, xkey, ssq, rstd, tmp1, xn, tag, on_dve=False):
                if on_dve:
                    A("dve", lambda e: e.scalar_tensor_tensor(out=junk3[:], in0=xt, scalar=1.0, in1=xt, op0=ALU.mult, op1=ALU.mult,
                                                              accum_out=ssq),
                      r=[xkey], w=["junk3", tag + "ssq"])
                else:
                    A("act", lambda e: e.activation(out=junk[:], in_=xt, func=AF.Square, accum_out=ssq),
                      r=[xkey], w=["junk", tag + "ssq"])
                rsq(rstd, ssq, 1, 1.0 / D, EPS, tmp1, [tag + "ssq"], tag + "rstd")
                A("dve", lambda e: e.tensor_scalar(out=xn, in0=xt, scalar1=rstd, scalar2=None, op0=ALU.mult),
                  r=[xkey, tag + "rstd"], w=[tag + "xn"])

            def fm_transposes(xn, xnkey, bank):
                for k in range(8):
                    A("pe", lambda e, k=k: e.transpose(out=PSB[bank][:, k * 128:(k + 1) * 128],
                                                       in_=xn[:, k * 128:(k + 1) * 128], identity=identb[:]),
                      r=[xnkey, "identb"], w=[pk(bank)])

            def to_featmajor(xn, xnkey, bank, scale3, bias3, j, hxT, hkey, do_transposes=True, dve_ks=()):
                if do_transposes:
                    fm_transposes(xn, xnkey, bank)
                for k in range(8):
                    if k in dve_ks:
                        A("dve", lambda e, k=k: e.tensor_scalar(out=hxT[:, k, :], in0=PSB[bank][:, k * 128:(k + 1) * 128],
                                                                scalar1=scale3(k, j), scalar2=bias3(k, j), op0=ALU.mult, op1=ALU.add),
                          r=[pk(bank), "smix", "mod", "sffn"], w=[hkey])
                    else:
                        A("act", lambda e, k=k: e.activation(out=hxT[:, k, :], in_=PSB[bank][:, k * 128:(k + 1) * 128],
                                                             func=AF.Identity, scale=scale3(k, j), bias=bias3(k, j)),
                          r=[pk(bank), "smix", "mod", "sffn"], w=[hkey])

            def qknorm_rope_gen(src, srckey, goff, rope, cos_t, sin_t, ckeys, T, outb, okey, tag, engs=("pool",) * 5):
                t0, t1, t2, st8, st8b, st8t = T
                pv = src.rearrange("p (g d) -> p g d", g=8)
                t1v = t1.rearrange("p (g d) -> p g d", g=8)
                t1k = tag + "t1"
                A("act", lambda e: e.activation(out=t0, in_=src, func=AF.Square), r=[srckey], w=[tag + "t0"])
                yield
                A("dve", lambda e: e.tensor_reduce(out=st8, in_=t0.rearrange("p (g d) -> p g d", g=8), axis=AX.X, op=ALU.add),
                  r=[tag + "t0"], w=[tag + "st8"])
                A("dve", lambda e: e.tensor_scalar(out=st8t, in0=st8, scalar1=1.0 / 64, scalar2=EPS, op0=ALU.mult, op1=ALU.add),
                  r=[tag + "st8"], w=[tag + "st8b_t"])
                yield
                A("pool", lambda e: e.tensor_tensor(out=st8b, in0=st8t, in1=mhalf[:, 0:8], op=ALU.pow),
                  r=[tag + "st8b_t", "mhalf"], w=[tag + "st8b"])
                yield
                A("dve", lambda e: e.tensor_tensor(out=t1v, in0=pv, in1=st8b.unsqueeze(2).to_broadcast([128, 8, 64]), op=ALU.mult),
                  r=[srckey, tag + "st8b"], w=[t1k])
                gain = qkg[:, goff:goff + 64].unsqueeze(1).to_broadcast([128, 8, 64])
                if not rope:
                    A("dve", lambda e: e.tensor_tensor(out=outb.rearrange("p (g d) -> p g d", g=8), in0=t1v, in1=gain, op=ALU.mult),
                      r=[t1k, "qkg"], w=[okey])
                    yield
                    return
                yield
                A(engs[0], lambda e: e.tensor_tensor(out=t1v, in0=t1v, in1=gain, op=ALU.mult), r=[t1k, "qkg"], w=[t1k])
                x1 = t1v[:, :, 0::2]
                x2 = t1v[:, :, 1::2]
                cb = cos_t.unsqueeze(1).to_broadcast([128, 8, 32])
                sbb = sin_t.unsqueeze(1).to_broadcast([128, 8, 32])
                t2v = t2.rearrange("p (i g d) -> p i g d", i=4, g=8)
                ov = outb.rearrange("p (g d) -> p g d", g=8)
                A(engs[1], lambda e: e.tensor_tensor(out=t2v[:, 0], in0=x1, in1=cb, op=ALU.mult), r=[t1k] + ckeys, w=[tag + "ra"])
                A(engs[2], lambda e: e.tensor_tensor(out=t2v[:, 1], in0=x2, in1=sbb, op=ALU.mult), r=[t1k] + ckeys, w=[tag + "rb"])
                yield
                A("dve", lambda e: e.tensor_tensor(out=ov[:, :, 0::2], in0=t2v[:, 0], in1=t2v[:, 1], op=ALU.subtract),
                  r=[tag + "ra", tag + "rb"], w=[okey])
                A(engs[3], lambda e: e.tensor_tensor(out=t2v[:, 2], in0=x1, in1=sbb, op=ALU.mult), r=[t1k] + ckeys, w=[tag + "rc"])
                A(engs[4], lambda e: e.tensor_tensor(out=t2v[:, 3], in0=x2, in1=cb, op=ALU.mult), r=[t1k] + ckeys, w=[tag + "rd"])
                yield
                A("dve", lambda e: e.tensor_tensor(out=ov[:, :, 1::2], in0=t2v[:, 2], in1=t2v[:, 3], op=ALU.add),
                  r=[tag + "rc", tag + "rd", okey], w=[okey])
                yield

            with ExitStack() as p1:
                wkv = sb("wkv", [128, 8, 2048], BF16, p1)
                gml = sb("gml", [128, 1536], F32, p1)
                wsT = sb("wsT", [128, 512], BF16, p1)
                load("sp", "gml", gml[:], gml_d)
                load("pool", "wsT", wsT[:], wsT_d)
                for i in range(4):
                    A("pool", lambda e, i=i: e.dma_start(
                        out=wkv[:, :, i * 512:(i + 1) * 512],
                        in_=win_d[:, 512 + i * 512:512 + (i + 1) * 512].rearrange("(k p) n -> p k n", p=128)),
                      w=[("wkv", i)], chan=("wkv", i))
                xt = [sb("xt%d" % i, [128, 1024], F32, p1) for i in range(2)]
                cst = [sb("cst%d" % i, [128, 64], F32, p1) for i in range(2)]
                xn = sb("xn", [128, 1024], BF16, p1)
                hxT = [sb("hxT%d" % i, [128, 8, 128], BF16, p1) for i in range(2)]
                st = sb("st", [128, 64], F32, p1)
                t0 = sb("t0", [128, 512], F32, p1)
                t1 = sb("t1", [128, 512], F32, p1)
                t2 = sb("t2", [128, 1024], F32, p1)
                kr = sb("kr", [128, 512], BF16, p1)
                us = sb("us", [128, 512], F32, p1)
                gs = sb("gs", [128, 512], F32, p1)
                vc = sb("vc", [128, 512], F32, p1)
                vn = sb("vn", [128, 512], BF16, p1)
                yy = sb("yy", [128, 512], F32, p1)
                ysq = sb("ysq", [128, 512], F32, p1)
                gmx = sb("gmx", [128, 512], BF16, p1)
                bst = sb("bst", [128, 4, 6], F32, p1)
                mv = sb("mv", [128, 4, 2], F32, p1)

                kraw = sb("kraw", [128, 512], F32, p1)
                kraw2 = sb("kraw2", [128, 512], F32, p1)
                vn2 = sb("vn2", [128, 512], BF16, p1)
                kr2 = sb("kr2", [128, 512], BF16, p1)
                krs = [kr, kr2]
                vns = [vn, vn2]
                kraws = [kraw, kraw2]
                t1b = sb("t1b", [128, 512], F32, p1)
                usb = sb("usb", [128, 512], F32, p1)
                gsb = sb("gsb", [128, 512], F32, p1)
                cst3 = sb("cst3", [128, 64], F32, p1)
                csts = [cst[0], cst[1], cst3]
                t1s = [t1, t1b]
                uss = [us, usb]
                gss = [gs, gsb]

                def b1_load_x(t):
                    bf = t % 2
                    src = ctx_d[t * 128:(t + 1) * 128, :] if t < 2 else x_d[(t - 2) * 128:(t - 1) * 128, :]
                    A("sp", lambda e: e.dma_start(out=xt[bf][:], in_=src), w=[("xt", bf)], chan=("xt", bf))

                def b1_load_cs(t):
                    cb3 = t % 3
                    if t >= 2:
                        r0 = (t - 2) * 128
                        A("sp", lambda e: e.dma_start(out=csts[cb3][:, 0:32], in_=cos_d[r0:r0 + 128, :]), w=[("cs", cb3, 0)], chan=("cs", cb3, 0))
                        A("sp", lambda e: e.dma_start(out=csts[cb3][:, 32:64], in_=sin_d[r0:r0 + 128, :]), w=[("cs", cb3, 1)], chan=("cs", cb3, 1))

                def Fa1_gen(t):
                    bf = t % 2
                    xnorm(xt[bf][:], ("xt", bf), st[:, 0:1], st[:, 1:2], st[:, 2:3], xn[:], "b1")
                    yield

                def Fa2_gen(t):
                    bf = t % 2
                    j = 1 if t < 2 else 0
                    fm_transposes(xn, "b1xn", 0)
                    yield
                    yield
                    to_featmajor(xn, "b1xn", 0, lambda k, j: smix3[:, k, j:j + 1], lambda k, j: mod3[:, k, j:j + 1], j,
                                 hxT[bf], ("hxT", bf), do_transposes=False)
                    yield

                def Fb_gen(t):
                    bf = t % 2
                    nproj = 2 if t < 2 else 4
                    for i in range(nproj):
                        for k in range(8):
                            A("pe", lambda e, i=i, k=k: e.matmul(PS[1 + i][:], lhsT=hxT[bf][:, k, :],
                                                                 rhs=wkv[:, k, i * 512:(i + 1) * 512],
                                                                 start=(k == 0), stop=(k == 7)),
                              r=[("hxT", bf), ("wkv", i)], w=[pk(1 + i)])
                        yield

                def K_gen(t):
                    par = t % 2
                    cb3 = t % 3
                    kraw_ = kraws[par]
                    A("act", lambda e: e.activation(out=kraw_[:], in_=PS[1][:], func=AF.Copy), r=[pk(1)], w=[("kraw", par)])
                    A("act", lambda e: e.activation(out=V1[:, t, :, 0:128],
                                                    in_=PS[2][:].rearrange("p (h d) -> p h d", h=4), func=AF.Copy),
                      r=[pk(2), "V1init"], w=[("V1", t)])
                    yield
                    yield from qknorm_rope_gen(kraw_[:], ("kraw", par), 64, t >= 2, csts[cb3][:, 0:32], csts[cb3][:, 32:64],
                                               [("cs", cb3, 0), ("cs", cb3, 1)],
                                               (t0[:], t1s[par][:], t2[:], st[:, 8:16], st[:, 16:24], st[:, 24:32]),
                                               krs[par][:], ("kr", par), "k%d" % par, engs=("pool", "pool", "dve", "pool", "dve"))

                def Kt_gen(t):
                    kr_ = krs[t % 2]
                    for h in range(4):
                        A("pe", lambda e, h=h: e.transpose(out=PSB[5][:, h * 128:(h + 1) * 128],
                                                           in_=kr_[:, h * 128:(h + 1) * 128], identity=identb[:]),
                          r=[("kr", t % 2), "identb"], w=[pk(5)])
                    yield
                    yield
                    A("act", lambda e: e.activation(out=KT[:, :, t * 128:(t + 1) * 128],
                                                    in_=PSB[5][:, 0:512].rearrange("p (h t) -> p h t", h=4), func=AF.Copy),
                      r=[pk(5)], w=[("KT", t)])
                    yield

                def G_gen(t):
                    par = t % 2
                    us_, gs_ = uss[par], gss[par]
                    uk, gk = ("us", par), ("gs", par)
                    A("act", lambda e: e.activation(out=us_[:], in_=PS[3][:], func=AF.Gelu), r=[pk(3)], w=[uk])
                    A("act", lambda e: e.activation(out=gs_[:], in_=PS[4][:], func=AF.Gelu), r=[pk(4)], w=[gk])
                    yield
                    for h in range(4):
                        A("dve", lambda e, h=h: e.bn_stats(out=bst[:, h, :], in_=gs_[:, h * 128:(h + 1) * 128]), r=[gk], w=[("bst", h)])
                    for h in range(4):
                        A("dve", lambda e, h=h: e.bn_aggr(out=mv[:, h, :], in_=bst[:, h, :]), r=[("bst", h)], w=[("mv", h)])
                    A("dve", lambda e: e.tensor_scalar(out=st[:, 36:40], in0=mv[:, :, 1], scalar1=1.0, scalar2=EPS, op0=ALU.mult, op1=ALU.add),
                      r=[("mv", h) for h in range(4)], w=["lnr_t"])
                    yield
                    A("pool", lambda e: e.tensor_tensor(out=st[:, 32:36], in0=st[:, 36:40], in1=mhalf[:, 0:4], op=ALU.pow),
                      r=["lnr_t", "mhalf"], w=["lnr"])
                    yield
                    for h in range(4):
                        A("dve", lambda e, h=h: e.tensor_scalar(out=vc[:, h * 128:(h + 1) * 128], in0=gs_[:, h * 128:(h + 1) * 128],
                                                                scalar1=mv[:, h, 0:1], scalar2=st[:, 32 + h:33 + h],
                                                                op0=ALU.subtract, op1=ALU.mult),
                          r=[gk, ("mv", h), "lnr"], w=[("vc", h)])
                    yield
                    A("pool", lambda e: e.tensor_tensor(out=vc[:], in0=vc[:], in1=gml[:, 0:512], op=ALU.mult),
                      r=[("vc", h) for h in range(4)] + ["gml"], w=[("vc", h) for h in range(4)])
                    vn_ = vns[par]
                    A("pool", lambda e: e.tensor_tensor(out=vn_[:], in0=vc[:], in1=gml[:, 512:1024], op=ALU.add),
                      r=[("vc", h) for h in range(4)] + ["gml"], w=[("vn", par)])
                    yield

                def G2_gen(t):
                    par = t % 2
                    us_ = uss[par]
                    uk = ("us", par)
                    vn_ = vns[par]
                    for h in range(4):
                        A("pe", lambda e, h=h: e.matmul(PS[6][:, h * 128:(h + 1) * 128], lhsT=wsT[:, h * 128:(h + 1) * 128],
                                                        rhs=vn_[:, h * 128:(h + 1) * 128], start=True, stop=True),
                          r=["wsT", ("vn", par)], w=[pk(6)])
                    yield
                    yield
                    for h in range(4):
                        A("dve", lambda e, h=h: e.scalar_tensor_tensor(
                            out=yy[:, h * 128:(h + 1) * 128], in0=PS[6][:, h * 128:(h + 1) * 128], scalar=bsT[:, h:h + 1],
                            in1=us_[:, h * 128:(h + 1) * 128], op0=ALU.add, op1=ALU.mult),
                          r=[pk(6), "bsT", uk], w=[("yy", h)])
                    yield
                    for h in range(4):
                        A("dve", lambda e, h=h: e.scalar_tensor_tensor(
                            out=ysq[:, h * 128:(h + 1) * 128], in0=yy[:, h * 128:(h + 1) * 128], scalar=1.0,
                            in1=yy[:, h * 128:(h + 1) * 128], op0=ALU.mult, op1=ALU.mult, accum_out=st[:, 40 + h:41 + h]),
                          r=[("yy", h)], w=[("ysq", h), ("yss", h)])
                    A("dve", lambda e: e.tensor_scalar(out=st[:, 48:52], in0=st[:, 40:44], scalar1=1.0 / 128, scalar2=EPS, op0=ALU.mult, op1=ALU.add),
                      r=[("yss", h) for h in range(4)], w=["yr_t"])
                    yield
                    A("pool", lambda e: e.tensor_tensor(out=st[:, 44:48], in0=st[:, 48:52], in1=mhalf[:, 0:4], op=ALU.pow),
                      r=["yr_t", "mhalf"], w=["yr"])
                    yield
                    A("dve", lambda e: e.tensor_tensor(out=yy[:].rearrange("p (g d) -> p g d", g=4),
                                                       in0=yy[:].rearrange("p (g d) -> p g d", g=4),
                                                       in1=st[:, 44:48].unsqueeze(2).to_broadcast([128, 4, 128]), op=ALU.mult),
                      r=[("yy", h) for h in range(4)] + ["yr"], w=[("yy", h) for h in range(4)])
                    yield
                    A("pool", lambda e: e.tensor_tensor(out=gmx[:], in0=yy[:], in1=gml[:, 1024:1536], op=ALU.mult),
                      r=[("yy", h) for h in range(4)] + ["gml"], w=["gmx"])
                    yield

                def G3_gen(t):
                    for h in range(4):
                        A("pe", lambda e, h=h: e.transpose(out=PSB[7][:, h * 128:(h + 1) * 128],
                                                           in_=gmx[:, h * 128:(h + 1) * 128], identity=identb[:]),
                          r=["gmx", "identb"], w=[pk(7)])
                    yield
                    yield
                    tt = t - 2
                    A("act", lambda e: e.activation(out=gmT[:, :, tt * 128:(tt + 1) * 128],
                                                    in_=PSB[7][:, 0:512].rearrange("p (h t) -> p h t", h=4), func=AF.Copy),
                      r=[pk(7)], w=[("gmT", tt)])
                    yield

                b1_load_x(0)
                b1_load_x(1)
                b1_load_cs(2)
                round_robin([Fa1_gen(0)])
                b1_load_x(2)
                round_robin([Fa2_gen(0), Fa1_gen(1)])
                b1_load_x(3)
                round_robin([Fb_gen(0), Fa2_gen(1), Fa1_gen(2)])
                def delayed(gen, n):
                    for _ in range(n):
                        yield
                    yield from gen

                for t in range(NKC + 2):
                    tails = []
                    if 0 <= t - 1 < NKC:
                        tails.append(Kt_gen(t - 1))
                    if 2 <= t - 2 < NKC:
                        tails.append(G3_gen(t - 2))
                    if 2 <= t - 1 < NKC:
                        tails.append(G2_gen(t - 1))
                    if t >= NKC:
                        round_robin(tails)
                        continue
                    gens = [K_gen(t)]
                    if t >= 2:
                        gens.append(G_gen(t))
                    if t + 1 < NKC:
                        gens.append(Fb_gen(t + 1))
                    if t + 2 < NKC:
                        gens.append(Fa2_gen(t + 2))
                    if t + 3 < NKC:
                        gens.append(Fa1_gen(t + 3))
                    gens += [delayed(g_, 4) for g_ in tails]
                    round_robin(gens)
                    if t + 4 < NKC:
                        b1_load_x(t + 4)
                    if t + 3 < NKC:
                        b1_load_cs(t + 3)
                if debug:
                    A("pool", lambda e: e.dma_start(out=dbg["d_kt"], in_=KT[:].rearrange("p h t -> p (h t)")),
                      r=[("KT", t) for t in range(NKC)], chan="d_kt")
                    A("pool", lambda e: e.dma_start(out=dbg["d_v1"], in_=V1[:].rearrange("p a b c -> p (a b c)")),
                      r=[("V1", t) for t in range(NKC)], chan="d_v1")
                    A("pool", lambda e: e.dma_start(out=dbg["d_gmt"], in_=gmT[:].rearrange("p h t -> p (h t)")),
                      r=[("gmT", t) for t in range(NT)], chan="d_gmt")
                S.emit()

            with ExitStack() as p2:
                wq = sb("wq", [128, 8, 512], BF16, p2)
                wo = sb("wo", [128, 8, 1024], BF16, p2)
                A("pool", lambda e: e.dma_start(out=wq[:], in_=win_d[:, 0:512].rearrange("(k p) n -> p k n", p=128)),
                  w=["wq"], chan="wq")
                for i in range(2):
                    A("pool", lambda e, i=i: e.dma_start(out=wo[:, :, i * 512:(i + 1) * 512],
                                                         in_=wout_d[:, i * 512:(i + 1) * 512].rearrange("(k p) n -> p k n", p=128)),
                      w=[("wo", i)], chan=("wo", i))
                subg = sb("subg", [128, 128], F32, p2)
                load("sp", "subg", subg[:], subg_d)
                gm_b = sb("gm_b", [128, 1024], F32, p2)
                bcast_rows(gm_b, 16, "gm_b")
                xt = [sb("xq%d" % i, [128, 1024], F32, p2) for i in range(2)]
                xo = sb("xo", [128, 1024], F32, p2)
                cst = [sb("csq%d" % i, [128, 64], F32, p2) for i in range(2)]
                xn = sb("xnq", [128, 1024], BF16, p2)
                hxT = sb("hxTq", [128, 8, 128], BF16, p2)
                st = sb("stq", [128, 64], F32, p2)
                t0 = sb("t0q", [128, 512], F32, p2)
                t1 = sb("t1q", [128, 512], F32, p2)
                t2 = sb("t2q", [128, 1024], F32, p2)
                qr = sb("qr", [128, 512], BF16, p2)
                QTa = [sb("QTa%d" % i, [128, 4, 512], BF16, p2) for i in range(2)]
                pt1 = [sb("pt1_%d" % i, [128, 512], BF16, p2) for i in range(3)]
                pt2 = [sb("pt2_%d" % i, [128, 512], BF16, p2) for i in range(3)]
                ax = [sb("ax%d" % i, [128, 4, 512], BF16, p2) for i in range(2)]
                axT = sb("axT", [128, 4, 128], BF16, p2)
                x1t = sb("x1t", [128, 1024], F32, p2)
                ot = sb("ot", [128, 4, 128], F32, p2)
                oo = sb("oo", [128, 4, 128], F32, p2)
                pst = sb("pst", [128, 8, 8], F32, p2)

                def acc(m, s):
                    i = m * 4 + s
                    bank = 5 + i // 3
                    c0 = (i % 3) * 130
                    return bank, PS[bank][:, c0:c0 + 129]

                nload = [0]

                def prep_gen(qt):
                    qb = qt % 2
                    for s in range(4):
                        tok0 = qt * 512 + s * 128
                        bf = nload[0] % 2
                        nload[0] += 1
                        A("sp", lambda e, bf=bf, tok0=tok0: e.dma_start(out=xt[bf][:], in_=x_d[tok0:tok0 + 128, :]),
                          w=[("xq", bf)], chan=("xq", bf))
                        A("sp", lambda e, bf=bf, tok0=tok0: e.dma_start(out=cst[bf][:, 0:32], in_=cos_d[tok0:tok0 + 128, :]),
                          w=[("csq", bf, 0)], chan=("csq", bf, 0))
                        A("sp", lambda e, bf=bf, tok0=tok0: e.dma_start(out=cst[bf][:, 32:64], in_=sin_d[tok0:tok0 + 128, :]),
                          w=[("csq", bf, 1)], chan=("csq", bf, 1))
                        yield
                        xnorm(xt[bf][:], ("xq", bf), st[:, 0:1], st[:, 1:2], st[:, 2:3], xn[:], "b2", on_dve=True)
                        for _ in range(8):
                            yield
                        fm_transposes(xn, "b2xn", 4)
                        for _ in range(3):
                            yield
                        to_featmajor(xn, "b2xn", 4, lambda k, j: smix3[:, k, j:j + 1], lambda k, j: mod3[:, k, j:j + 1], 0,
                                     hxT, "hxTq", do_transposes=False, dve_ks=tuple(range(8)))
                        for _ in range(4):
                            yield
                        for k in range(8):
                            A("pe", lambda e, k=k: e.matmul(PS[4][:], lhsT=hxT[:, k, :], rhs=wq[:, k, :],
                                                            start=(k == 0), stop=(k == 7)),
                              r=["hxTq", "wq"], w=[pk(4)])
                        for _ in range(4):
                            yield
                        for _ in qknorm_rope_gen(PS[4][:], pk(4), 0, True, cst[bf][:, 0:32], cst[bf][:, 32:64],
                                                 [("csq", bf, 0), ("csq", bf, 1)],
                                                 (t0[:], t1[:], t2[:], st[:, 8:16], st[:, 16:24], st[:, 24:32]), qr[:], "qr", "q",
                                                 engs=("dve",) * 5):
                            yield
                            yield
                            yield
                        for h in range(4):
                            A("pe", lambda e, h=h: e.transpose(out=PSB[4][:, h * 128:(h + 1) * 128],
                                                               in_=qr[:, h * 128:(h + 1) * 128], identity=identb[:]),
                              r=["qr", "identb"], w=[pk(4)])
                        for _ in range(3):
                            yield
                        pq = PSB[4][:, 0:512].rearrange("p (h t) -> p h t", h=4)
                        A("dve", lambda e, s=s, pq=pq: e.tensor_copy(out=QTa[qb][:, :, s * 128:(s + 1) * 128], in_=pq),
                          r=[pk(4)], w=[("QTa", qb, s)])
                        yield

                def oproj_gen(qt):
                    qb = qt % 2
                    for s in range(4):
                        tok0 = qt * 512 + s * 128
                        tile = qt * 4 + s
                        A("sp", lambda e, tok0=tok0: e.dma_start(out=xo[:], in_=x_d[tok0:tok0 + 128, :]), w=["xo"], chan="xo")
                        for h in range(4):
                            A("pe", lambda e, h=h, s=s: e.transpose(out=PSB[4][:, h * 128:(h + 1) * 128],
                                                                    in_=ax[qb][:, s, h * 128:(h + 1) * 128], identity=identb[:]),
                              r=[("ax", qb, s, h), "identb"], w=[pk(4)])
                        for _ in range(3):
                            yield
                        A("dve", lambda e: e.tensor_copy(out=axT[:], in_=PSB[4][:, 0:512].rearrange("p (h t) -> p h t", h=4)),
                          r=[pk(4)], w=["axT"])
                        for _ in range(3):
                            yield
                        for q4 in range(4):
                            for k in range(8):
                                if k < 4:
                                    lhs = axT[:, k, :]
                                    rk = "axT"
                                else:
                                    lhs = gmT[:, k - 4, tok0:tok0 + 128]
                                    rk = ("gmT", tile)
                                A("pe", lambda e, lhs=lhs, k=k, q4=q4: e.matmul(
                                    PS[4][:, 256:512], lhsT=lhs, rhs=wo[:, k, q4 * 256:(q4 + 1) * 256],
                                    start=(k == 0), stop=(k == 7)),
                                  r=[rk, ("wo", q4 // 2)], w=[pk(4)])
                            A("dve", lambda e, q4=q4: e.tensor_tensor(
                                out=x1t[:, q4 * 256:(q4 + 1) * 256], in0=PS[4][:, 256:512],
                                in1=gm_b[:, q4 * 256:(q4 + 1) * 256], op=ALU.mult),
                              r=[pk(4), "gm_b"], w=[("x1t", q4)])
                            yield
                            yield
                        A("pool", lambda e: e.tensor_tensor(out=x1t[:], in0=x1t[:], in1=xo[:], op=ALU.add),
                          r=[("x1t", q4) for q4 in range(4)] + ["xo"], w=[("x1t", q4) for q4 in range(4)])
                        A("sp", lambda e, tok0=tok0: e.dma_start(out=x1_d[tok0:tok0 + 128, :], in_=x1t[:]),
                          r=[("x1t", q4) for q4 in range(4)], w=[("x1s", tile)], chan="x1st")
                        yield

                def attention(qt, side):
                    qb = qt % 2
                    qa_keys = [("QTa", qb, s) for s in range(4)]
                    qb_keys = [("QTb", qb, s) for s in range(4)]
                    side = list(side)

                    def tick():
                        while side:
                            try:
                                next(side[0])
                                return
                            except StopIteration:
                                side.pop(0)

                    for h in range(4):
                        def qk(kc, h=h):
                            b = kc % 2
                            A("pe", lambda e: e.matmul(PS[b][:], lhsT=KT[0:64, h, kc * 128:(kc + 1) * 128], rhs=QTa[qb][0:64, h, :],
                                                       start=True, stop=True), r=[("KT", kc)] + qa_keys, w=[pk(b)])
                            A("pe", lambda e: e.matmul(PS[2 + b][:], lhsT=KT[64:128, h, kc * 128:(kc + 1) * 128], rhs=QTa[qb][64:128, h, :],
                                                       start=True, stop=True), r=[("KT", kc)] + qa_keys, w=[pk(2 + b)])
                            b3 = kc % 3
                            A("act", lambda e: e.activation(out=pt1[b3][:], in_=PS[b][:], func=AF.Exp, scale=0.125),
                              r=[pk(b)], w=[("pt1", b3)])
                            A("act", lambda e: e.activation(out=pt2[b3][:], in_=PS[2 + b][:], func=AF.Exp, scale=0.125),
                              r=[pk(2 + b)], w=[("pt2", b3)])

                        def pv(kc, h=h):
                            b = kc % 3
                            for m in range(2):
                                pt = pt1[b] if m == 0 else pt2[b]
                                for s in range(4):
                                    bank, ap = acc(m, s)
                                    first = (kc == 0) and ((m * 4 + s) % 3 == 0)
                                    A("pe", lambda e, pt=pt, s=s, ap=ap, first=first: e.matmul(
                                        ap, lhsT=pt[:, s * 128:(s + 1) * 128], rhs=V1[:, kc, h, 0:129],
                                        start=first, stop=(kc == NKC - 1), skip_group_check=True),
                                      r=[("pt1" if m == 0 else "pt2", b), ("V1", kc)], w=[pk(bank)])

                        qk(0)
                        qk(1)
                        for kc in range(NKC):
                            if kc + 2 < NKC:
                                qk(kc + 2)
                            pv(kc)
                            tick()
                            tick()
                        for s in range(4):
                            for m in range(2):
                                bk, a = acc(m, s)
                                A("dve", lambda e, s=s, m=m, a=a: e.reciprocal(out=pst[:, s, m:m + 1], in_=a[:, 128:129]),
                                  r=[pk(bk)], w=[("rr", s, m)])
                        for s in range(4):
                            A("dve", lambda e, s=s: e.tensor_tensor(out=pst[:, s, 2:3], in0=pst[:, s, 1:2], in1=nlam, op=ALU.mult),
                              r=[("rr", s, 1), "nlam"], w=[("rr", s, 2)])
                        for s in range(4):
                            bk, a1 = acc(0, s)
                            A("dve", lambda e, s=s, a1=a1: e.tensor_scalar(out=ot[:, s, :], in0=a1[:, 0:128], scalar1=pst[:, s, 0:1],
                                                                         scalar2=None, op0=ALU.mult),
                              r=[pk(bk), ("rr", s, 0)], w=[("ot", s)])
                        for s in range(4):
                            bk, a2 = acc(1, s)
                            A("dve", lambda e, s=s, a2=a2: e.scalar_tensor_tensor(out=oo[:, s, :], in0=a2[:, 0:128], scalar=pst[:, s, 2:3],
                                                                                 in1=ot[:, s, :], op0=ALU.mult, op1=ALU.add),
                              r=[pk(bk), ("rr", s, 2), ("ot", s)], w=[("oo", s)])
                        for s in range(4):
                            A("dve", lambda e, s=s: e.scalar_tensor_tensor(out=junk2[:, s * 128:(s + 1) * 128], in0=oo[:, s, :], scalar=1.0,
                                                                           in1=oo[:, s, :], op0=ALU.mult, op1=ALU.mult,
                                                                           accum_out=pst[:, s, 3:4]),
                              r=[("oo", s)], w=[("junk2", s), ("rr", s, 3)])
                        c = (1.0 - LAMBDA_INIT) ** 2
                        for s in range(4):
                            rsq(pst[:, s, 4:5], pst[:, s, 3:4], 1, 1.0 / (128 * c), EPS / c, pst[:, s, 5:6], [("rr", s, 3)], "rr4_%d" % s)
                        for s in range(4):
                            A("dve", lambda e, s=s, h=h: e.scalar_tensor_tensor(
                                out=ax[qb][:, s, h * 128:(h + 1) * 128], in0=oo[:, s, :], scalar=pst[:, s, 4:5], in1=subg[:],
                                op0=ALU.mult, op1=ALU.mult),
                              r=[("oo", s), "rr4_%d" % s, "subg"], w=[("ax", qb, s, h)])
                    while side:
                        tick()

                for _ in prep_gen(0):
                    pass
                for qt in range(8):
                    side = []
                    if qt >= 1:
                        side.append(oproj_gen(qt - 1))
                    if qt + 1 < 8:
                        side.append(prep_gen(qt + 1))
                    attention(qt, side)
                for _ in oproj_gen(7):
                    pass
                S.emit()

        with ExitStack() as pc:
            fxT = sb("fxT", [128, 8, 2048], BF16, pc)
            accm = sb("accm", [128, 16, 1024], F32, pc)
            gates = sb("gates", [128, 16, 32], F32, pc)
            xt = [sb("xc%d" % i, [128, 1024], F32, pc) for i in range(2)]
            xn = sb("xnc", [128, 1024], BF16, pc)
            st = sb("stc", [128, 16], F32, pc)
            wgu = [sb("wgu%d" % i, [128, 8, 512], BF16, pc) for i in range(2)]
            wd = [sb("wd%d" % i, [128, 2, 1024], BF16, pc) for i in range(2)]
            sg = [sb("sg%d" % i, [128, 512], BF16, pc) for i in range(2)]
            hmT = [sb("hmT%d" % i, [128, 2, 512], BF16, pc) for i in range(2)]
            rt = sb("rt", [128, 8, 32], F32, pc)
            m8 = sb("m8", [128, 6, 8], F32, pc)
            grp8 = sb("grp8", [128, 8], F32, pc)
            ofin = [sb("ofin%d" % i, [128, 1024], F32, pc) for i in range(2)]
            gf_b = sb("gf_b", [128, 1024], F32, pc)
            bcast_rows(gf_b, 40, "gf_b")
            A("dve", lambda e: e.memset(grp8[:], -1e30), w=["grp8"])
            out_ops = []
            NP = 4
            xt = xt + [sb("xc%d" % i, [128, 1024], F32, pc) for i in range(2, NP)]
            xn2 = [xn] + [sb("xnc%d" % i, [128, 1024], BF16, pc) for i in range(1, NP)]
            st2 = [st] + [sb("stc%d" % i, [128, 16], F32, pc) for i in range(1, NP)]
            rt2 = [rt] + [sb("rt%d" % i, [128, 8, 32], F32, pc) for i in range(1, NP)]
            m82 = [m8] + [sb("m8_%d" % i, [128, 6, 8], F32, pc) for i in range(1, NP)]
            grp82 = [grp8] + [sb("grp8_%d" % i, [128, 8], F32, pc) for i in range(1, NP)]
            xf = [sb("xf%d" % i, [128, 1024], F32, pc) for i in range(2)]
            for i in range(NP):
                A("dve", lambda e, i=i: e.memset(grp82[i][:], -1e30), w=[("grp8", i)])

            def P_gen(hf, t):
                q = t % NP
                tile = hf * 16 + t
                tok0 = tile * 128
                xt_, xn_, st_, rt_, m8_, g8_ = xt[q], xn2[q], st2[q], rt2[q], m82[q], grp82[q]
                b0, b1 = 2 * q, 2 * q + 1
                K = lambda nm: (nm, q)
                A("sp", lambda e: e.dma_start(out=xt_[:], in_=x1_d[tok0:tok0 + 128, :]),
                  r=[("x1s", tile)], w=[("xc", q)], chan=("xc", q))
                yield
                A("act", lambda e: e.activation(out=junk[:], in_=xt_[:], func=AF.Square, accum_out=st_[:, 0:1]),
                  r=[("xc", q)], w=["junk", K("ssq")])
                yield
                A("dve", lambda e: e.tensor_scalar(out=st_[:, 2:3], in0=st_[:, 0:1], scalar1=1.0 / D, scalar2=EPS, op0=ALU.mult, op1=ALU.add),
                  r=[K("ssq")], w=[K("rstd_t")])
                yield
                A("pool", lambda e: e.tensor_tensor(out=st_[:, 1:2], in0=st_[:, 2:3], in1=mhalf[:, 0:1], op=ALU.pow),
                  r=[K("rstd_t"), "mhalf"], w=[K("rstd")])
                yield
                A("dve", lambda e: e.tensor_scalar(out=xn_[:], in0=xt_[:], scalar1=st_[:, 1:2], scalar2=None, op0=ALU.mult),
                  r=[("xc", q), K("rstd")], w=[K("xn")])
                yield
                for k in range(8):
                    A("pe", lambda e, k=k: e.transpose(out=PSB[b0][:, k * 128:(k + 1) * 128],
                                                       in_=xn_[:, k * 128:(k + 1) * 128], identity=identb[:]),
                      r=[K("xn"), "identb"], w=[pk(b0)])
                yield
                for k in range(8):
                    if k % 2 == 1:
                        A("dve", lambda e, k=k: e.tensor_scalar(out=fxT[:, k, t * 128:(t + 1) * 128],
                                                                in0=PSB[b0][:, k * 128:(k + 1) * 128],
                                                                scalar1=sffn[:, k:k + 1], scalar2=mod3[:, 24 + k, 0:1],
                                                                op0=ALU.mult, op1=ALU.add),
                          r=[pk(b0), "sffn", "mod"], w=[("fxT", t)])
                    else:
                        A("act", lambda e, k=k: e.activation(out=fxT[:, k, t * 128:(t + 1) * 128],
                                                             in_=PSB[b0][:, k * 128:(k + 1) * 128], func=AF.Identity,
                                                             scale=sffn[:, k:k + 1], bias=mod3[:, 24 + k, 0:1]),
                          r=[pk(b0), "sffn", "mod"], w=[("fxT", t)])
                yield
                for k in range(8):
                    A("pe", lambda e, k=k: e.matmul(PS[b1][:, 0:32], lhsT=fxT[:, k, t * 128:(t + 1) * 128], rhs=wr[:, k, :],
                                                    start=(k == 0), stop=(k == 7)),
                      r=[("fxT", t), "wr"], w=[pk(b1)])
                yield
                sc = rt_[:, 0, :]
                bi = rt_[:, 1, :]
                b10 = rt_[:, 2, :]
                msk = rt_[:, 3, :]
                sel = rt_[:, 4, :]
                ww = rt_[:, 5, :]
                gmask = rt_[:, 6, 0:4]
                A("act", lambda e: e.activation(out=sc, in_=PS[b1][:, 0:32], func=AF.Sigmoid), r=[pk(b1)], w=[K("sc")])
                yield
                A("dve", lambda e: e.tensor_tensor(out=bi, in0=sc, in1=rbias[:], op=ALU.add), r=[K("sc"), "rbias"], w=[K("bi")])
                yield
                for gi in range(4):
                    A("dve", lambda e, gi=gi: e.max(out=m8_[:, gi, :], in_=bi[:, gi * 8:(gi + 1) * 8]), r=[K("bi")], w=[("m8", q, gi)])
                A("dve", lambda e: e.tensor_scalar(out=b10, in0=bi, scalar1=10.0, scalar2=None, op0=ALU.add), r=[K("bi")], w=[K("b10")])
                yield
                A("dve", lambda e: e.tensor_tensor(out=g8_[:, 0:4], in0=m8_[:, 0:4, 0], in1=m8_[:, 0:4, 1], op=ALU.add),
                  r=[("m8", q, gi) for gi in range(4)] + [("grp8", q)], w=[K("grp")])
                yield
                A("dve", lambda e: e.max(out=m8_[:, 4, :], in_=g8_[:]), r=[K("grp")], w=[("m8", q, 4)])
                yield
                A("dve", lambda e: e.tensor_scalar(out=gmask, in0=g8_[:, 0:4], scalar1=m8_[:, 4, 1:2], scalar2=None, op0=ALU.is_ge),
                  r=[K("grp"), ("m8", q, 4)], w=[K("gmask")])
                yield
                A("dve", lambda e: e.tensor_tensor(
                    out=msk.rearrange("p (g i) -> p g i", g=4), in0=b10.rearrange("p (g i) -> p g i", g=4),
                    in1=gmask.unsqueeze(2).to_broadcast([128, 4, 8]), op=ALU.mult), r=[K("b10"), K("gmask")], w=[K("msk")])
                yield
                A("dve", lambda e: e.max(out=m8_[:, 5, :], in_=msk), r=[K("msk")], w=[("m8", q, 5)])
                yield
                A("dve", lambda e: e.tensor_scalar(out=sel, in0=msk, scalar1=m8_[:, 5, 3:4], scalar2=None, op0=ALU.is_ge),
                  r=[K("msk"), ("m8", q, 5)], w=[K("sel")])
                yield
                A("dve", lambda e: e.scalar_tensor_tensor(out=ww, in0=sc, scalar=1.0, in1=sel, op0=ALU.mult, op1=ALU.mult,
                                                          accum_out=st_[:, 4:5]),
                  r=[K("sc"), K("sel")], w=[K("ww"), K("wsum")])
                yield
                A("dve", lambda e: e.reciprocal(out=st_[:, 5:6], in_=st_[:, 4:5]), r=[K("wsum")], w=[K("rws")])
                yield
                A("dve", lambda e: e.tensor_scalar(out=gates[:, t, :], in0=ww, scalar1=st_[:, 5:6], scalar2=2.5,
                                                   op0=ALU.mult, op1=ALU.mult),
                  r=[K("ww"), K("rws")], w=[("gates", t)])
                yield

            def Fin_gen(hf, t):
                q = t % 2
                tile = hf * 16 + t
                tok0 = tile * 128
                A("sp", lambda e: e.dma_start(out=xf[q][:], in_=x1_d[tok0:tok0 + 128, :]),
                  r=[("x1s", tile)], w=[("xf", q)], chan=("xf", q))
                yield
                A("dve", lambda e: e.tensor_tensor(out=ofin[q][:], in0=accm[:, t, :], in1=gf_b[:], op=ALU.mult),
                  r=[("accm", t, 0), ("accm", t, 1), "gf_b"], w=[("ofin", q)])
                yield
                A("pool", lambda e: e.tensor_tensor(out=ofin[q][:], in0=ofin[q][:], in1=xf[q][:], op=ALU.add),
                  r=[("ofin", q), ("xf", q)], w=[("ofin", q)])
                yield
                out_ops.append(A("sp", lambda e: e.dma_start(out=out_d[tok0:tok0 + 128, :], in_=ofin[q][:]),
                                 r=[("ofin", q)], chan=("ost", q)))
                yield

            def seq(*gens):
                for g_ in gens:
                    yield from g_

            for i in range(4):
                round_robin([P_gen(0, 4 * i + j) for j in range(4)])
            for hf in range(2):
                fx_keys = [("fxT", t) for t in range(16)]

                def wload(e_i):
                    bf = e_i % 2
                    if e_i < NEXP:
                        sg_, su_, sd_ = weg_d[e_i], weu_d[e_i], wed_d[e_i]
                    else:
                        sg_, su_, sd_ = wsg_d, wsu_d, wsd_d
                    A("pool", lambda e: e.dma_start(out=wgu[bf][:, :, 0:256], in_=sg_.rearrange("(k p) f -> p k f", p=128)),
                      w=[("wg", bf)], chan=("wg", bf))
                    A("pool", lambda e: e.dma_start(out=wgu[bf][:, :, 256:512], in_=su_.rearrange("(k p) f -> p k f", p=128)),
                      w=[("wu", bf)], chan=("wu", bf))
                    A("pool", lambda e: e.dma_start(out=wd[bf][:], in_=sd_.rearrange("(k p) n -> p k n", p=128)),
                      w=[("wd", bf)], chan=("wd", bf))

                if hf == 0:
                    wload(0)
                    wload(1)
                dcount = [0]

                def gu_group(e_i, tc, g):
                    bf = e_i % 2
                    hb = (e_i * 4 + tc) % 2
                    mat, fc = divmod(g, 2)
                    bank = g
                    for k in range(8):
                        A("pe", lambda e, mat=mat, fc=fc, k=k, bank=bank: e.matmul(
                            PS[bank][:], lhsT=wgu[bf][:, k, mat * 256 + fc * 128:mat * 256 + (fc + 1) * 128],
                            rhs=fxT[:, k, tc * 512:(tc + 1) * 512], start=(k == 0), stop=(k == 7)),
                          r=[("wg" if mat == 0 else "wu", bf)] + fx_keys[tc * 4:(tc + 1) * 4], w=[pk(bank)])
                    if mat == 0:
                        A("act", lambda e, fc=fc: e.activation(out=sg[fc][:], in_=PS[fc][:], func=AF.Silu), r=[pk(fc)], w=[("sg", fc)])
                    else:
                        A("dve", lambda e, fc=fc: e.tensor_tensor(out=hmT[hb][:, fc, :], in0=sg[fc][:], in1=PS[2 + fc][:], op=ALU.mult),
                          r=[("sg", fc), pk(2 + fc)], w=[("hmT", hb, fc)])

                def gu(e_i, tc):
                    for g in range(4):
                        gu_group(e_i, tc, g)

                def down(e_i, tc, nxt=None):
                    bf = e_i % 2
                    hb = (e_i * 4 + tc) % 2
                    for s in range(4):
                        if nxt is not None:
                            gu_group(nxt[0], nxt[1], s)
                        t = tc * 4 + s
                        db = dcount[0] % 2
                        dcount[0] += 1
                        for half in range(2):
                            bank = 4 + db * 2 + half
                            for fc in range(2):
                                A("pe", lambda e, fc=fc, half=half, bank=bank, s=s: e.matmul(
                                    PS[bank][:], lhsT=hmT[hb][:, fc, s * 128:(s + 1) * 128],
                                    rhs=wd[bf][:, fc, half * 512:(half + 1) * 512], start=(fc == 0), stop=(fc == 1)),
                                  r=[("hmT", hb, fc), ("wd", bf)], w=[pk(bank)])
                            dst = accm[:, t, half * 512:(half + 1) * 512]
                            if e_i == 0:
                                A("dve", lambda e, dst=dst, bank=bank, t=t: e.tensor_scalar(
                                    out=dst, in0=PS[bank][:], scalar1=gates[:, t, e_i:e_i + 1], scalar2=None, op0=ALU.mult),
                                  r=[pk(bank), ("gates", t)], w=[("accm", t, half)])
                            elif e_i < NEXP:
                                A("dve", lambda e, dst=dst, bank=bank, t=t: e.scalar_tensor_tensor(
                                    out=dst, in0=PS[bank][:], scalar=gates[:, t, e_i:e_i + 1], in1=dst, op0=ALU.mult, op1=ALU.add),
                                  r=[pk(bank), ("gates", t), ("accm", t, half)], w=[("accm", t, half)])
                            else:
                                A("dve", lambda e, dst=dst, bank=bank: e.tensor_tensor(out=dst, in0=PS[bank][:], in1=dst, op=ALU.add),
                                  r=[pk(bank), ("accm", t, half)], w=[("accm", t, half)])

                units = [(e_i, tc) for e_i in range(NEXP + 1) for tc in range(4)]
                gu(*units[0])
                for i, u in enumerate(units):
                    down(u[0], u[1], units[i + 1] if i + 1 < len(units) else None)
                    if u[1] == 3 and u[0] + 2 <= NEXP:
                        wload(u[0] + 2)
                    if hf == 1 and u[0] == NEXP:
                        tc_ = u[1]
                        round_robin([Fin_gen(1, tc_ * 4), Fin_gen(1, tc_ * 4 + 1)])
                        round_robin([Fin_gen(1, tc_ * 4 + 2), Fin_gen(1, tc_ * 4 + 3)])
                if hf == 0:
                    wload(0)
                    wload(1)
                    for i in range(4):
                        round_robin([seq(Fin_gen(0, 4 * i), Fin_gen(0, 4 * i + 2)), seq(Fin_gen(0, 4 * i + 1), Fin_gen(0, 4 * i + 3))]
                                    + [P_gen(1, 4 * i + j) for j in range(4)])
                else:
                    pass
            extra = list(out_ops) + list(S.chan_last.values())
            A("sp", lambda e: e.nop(), extra=extra)
            S.emit()
    return nc


def rope_tables():
    rows = L // 64
    row = np.repeat(np.arange(rows, dtype=np.float32), 64)
    col = np.tile(np.arange(64, dtype=np.float32), rows)
    half = 32
    inv_freq = (np.float32(10000.0) ** (-np.arange(0, half, 2, dtype=np.float32) / np.float32(half))).astype(np.float32)
    ang = np.concatenate([row[:, None] * inv_freq, col[:, None] * inv_freq], axis=-1).astype(np.float32)
    return np.cos(ang).astype(np.float32), np.sin(ang).astype(np.float32)


def make_in_maps(inp, cores):
    f = lambda a: np.ascontiguousarray(np.asarray(a, dtype=np.float32))
    cos, sin = rope_tables()
    bc = lambda v: f(np.broadcast_to(np.asarray(v, np.float32).reshape(1, -1), (128, np.asarray(v).size)))
    col = lambda v, n: f(np.asarray(v, np.float32).reshape(n, 128).T)
    shared = {
        "w_ada": f(inp["w_ada"][0]),
        "b_adaT": col(inp["b_ada"][0], 48),
        "nmg": col(inp["norm_mix_g"][0], 8),
        "nfg": col(inp["norm_ffn_g"][0], 8),
        "w_in": f(inp["w_in"][0]),
        "qkg": bc(np.concatenate([np.asarray(inp["q_norm_g"][0]), np.asarray(inp["k_norm_g"][0])])),
        "lam_in": bc(np.asarray(inp["da_lambda"][0]).reshape(-1)),
        "subg": bc(inp["subln_g"][0]),
        "gml": bc(np.concatenate([np.asarray(inp["gm_ln_g"][0]).reshape(-1), np.asarray(inp["gm_ln_b"][0]).reshape(-1),
                                  np.asarray(inp["gm_out_g"][0]).reshape(-1)])),
        "w_sT": f(np.transpose(np.asarray(inp["gm_ws"][0]), (2, 0, 1)).reshape(128, 512)),
        "bsT": f(np.asarray(inp["gm_bs"][0]).T),
        "w_out": f(inp["w_out"][0]),
        "w_router": f(inp["w_router"][0]),
        "rbias": bc(inp["router_bias"][0]),
        "we_gate": f(inp["we_gate"][0]),
        "we_up": f(inp["we_up"][0]),
        "we_down": f(inp["we_down"][0]),
        "ws_gate": f(inp["ws_gate"][0]),
        "ws_up": f(inp["ws_up"][0]),
        "ws_down": f(inp["ws_down"][0]),
        "cos": cos,
        "sin": sin,
        "ident": np.eye(128, dtype=np.float32),
    }
    maps = []
    cvec = np.asarray(inp["c"], np.float32)
    cctx = np.asarray(inp["c_ctx"], np.float32)
    for b in cores:
        m = dict(shared)
        m["x"] = f(inp["x"][b])
        m["ctx"] = f(inp["ctx"][b])
        two = np.stack([cvec[b], cctx], axis=-1).reshape(8, 128, 2)
        m["cc"] = f(np.transpose(two, (1, 0, 2)).reshape(128, 16))
        maps.append(m)
    return maps


_NC_CACHE = {}


def kernel(**inputs):
    if "nc" not in _NC_CACHE:
        _NC_CACHE["nc"] = build_program()
    nc = _NC_CACHE["nc"]
    cores = list(range(8))
    in_maps = make_in_maps(inputs, cores)
    res = run_bass_kernel_spmd(nc, in_maps, core_ids=cores)
    out = np.stack([np.asarray(res.results[b]["out"], dtype=np.float32) for b in cores], axis=0)
    return out
```
